# Optimizing a Trainium2 kernel written in Bass

```python
import jax, jax.numpy as jnp
from jax import lax
import numpy as np

D_MODEL = 1024
BATCH = 32
SEQ = 2048
DEPTH = 4

D_CONV_A = 512
CONV_A_WIDTH = 3
D_CONV_B = 512
CONV_B_WIDTH = 31
N_HEADS = 16
HEAD_DIM = 64
D_ATTN = N_HEADS * HEAD_DIM
BLOCK_Q = 128
N_GROUPS = 4
EXPERTS_PER_GROUP = 8
N_EXPERTS = N_GROUPS * EXPERTS_PER_GROUP
TOP_K = 2
D_EXPERT = 512
ROW_CHUNK = 256
LN_EPS = 1e-5
DEEPNORM_ALPHA = (2 * DEPTH) ** 0.25
DEEPNORM_BETA = (8 * DEPTH) ** -0.25

N_EVEN = (DEPTH + 1) // 2
N_ODD = DEPTH // 2
D_IN_EVEN = 3 * D_CONV_A + 2 * D_CONV_B
D_MIX_EVEN = D_CONV_A + D_CONV_B

kernel_name = "hybrid_conv_stickbreak_hmoe_deepnorm"


def layer_norm(x, g, b):
    xf = x.astype(jnp.float32)
    mu = jnp.mean(xf, axis=-1, keepdims=True)
    var = jnp.mean(jnp.square(xf - mu), axis=-1, keepdims=True)
    y = (xf - mu) * lax.rsqrt(var + LN_EPS)
    return (y * g + b).astype(x.dtype)


def causal_depthwise_conv(x, w):
    k = w.shape[0]
    return lax.conv_general_dilated(
        x, w[:, None, :].astype(x.dtype), window_strides=(1,), padding=[(k - 1, 0)],
        dimension_numbers=("NWC", "WIO", "NWC"), feature_group_count=x.shape[-1])


def conv_mixers(x, w_in, conv_a, conv_b_w, conv_b_bias, norm_b_g, norm_b_b, w_out):
    u = jnp.einsum("bsd,de->bse", x, w_in)
    a_b, a_c, a_v, b_val, b_gate = jnp.split(
        u, [D_CONV_A, 2 * D_CONV_A, 3 * D_CONV_A, 3 * D_CONV_A + D_CONV_B], axis=-1)
    y_a = a_b * causal_depthwise_conv(a_c * a_v, conv_a)
    g = b_val * jax.nn.sigmoid(b_gate)
    g = causal_depthwise_conv(g, conv_b_w) + conv_b_bias
    y_b = jax.nn.silu(layer_norm(g, norm_b_g, norm_b_b))
    return jnp.einsum("bse,ed->bsd", jnp.concatenate([y_a, y_b], axis=-1), w_out)


def stick_breaking_attention(x, w_qkv, w_o):
    b, s, _ = x.shape
    qkv = jnp.einsum("bsd,de->bse", x, w_qkv).reshape(b, s, 3, N_HEADS, HEAD_DIM)
    q = qkv[:, :, 0].transpose(0, 2, 1, 3)
    k = qkv[:, :, 1].transpose(0, 2, 1, 3)
    v = qkv[:, :, 2].transpose(0, 2, 1, 3)
    scale = HEAD_DIM ** -0.5
    outs = []
    for blk in range(s // BLOCK_Q):
        q0 = blk * BLOCK_Q
        kv_len = q0 + BLOCK_Q
        qb = q[:, :, q0:kv_len]
        kb = k[:, :, :kv_len]
        vb = v[:, :, :kv_len]
        z = jnp.einsum("bhqd,bhkd->bhqk", qb, kb,
                       preferred_element_type=jnp.float32) * scale
        t_idx = q0 + jnp.arange(BLOCK_Q)[:, None]
        s_idx = jnp.arange(kv_len)[None, :]
        causal = s_idx < t_idx
        log_keep = jnp.where(causal, jax.nn.log_sigmoid(-z), 0.0)
        later = lax.cumsum(log_keep, axis=3, reverse=True) - log_keep
        a = jnp.where(causal, jnp.exp(jax.nn.log_sigmoid(z) + later), 0.0)
        outs.append(jnp.einsum("bhqk,bhkd->bhqd", a.astype(vb.dtype), vb))
    o = jnp.concatenate(outs, axis=2).transpose(0, 2, 1, 3).reshape(b, s, D_ATTN)
    return jnp.einsum("bse,ed->bsd", o, w_o)


def hierarchical_moe(x, rg_w, rg_b, re_w, re_b, w_up, w_down):
    b, s, d = x.shape
    h = x.reshape(-1, d)
    t = h.shape[0]
    g_logits = jnp.matmul(h, rg_w).astype(jnp.float32) + rg_b
    grp = jnp.argmax(g_logits, axis=-1)
    g_gate = jnp.take_along_axis(jax.nn.softmax(g_logits, axis=-1), grp[:, None], axis=-1)
    e_logits = (jnp.matmul(h, re_w).astype(jnp.float32) + re_b).reshape(t, N_GROUPS, EXPERTS_PER_GROUP)
    e_logits = jnp.take_along_axis(e_logits, grp[:, None, None], axis=1)[:, 0]
    top_val, top_idx = lax.top_k(e_logits, TOP_K)
    weights = (g_gate * jax.nn.softmax(top_val, axis=-1)).reshape(-1)
    ids = (grp[:, None] * EXPERTS_PER_GROUP + top_idx).reshape(-1).astype(jnp.int32)
    n_assign = t * TOP_K
    order = jnp.argsort(ids)
    sorted_ids = ids[order]
    tok = order // TOP_K
    sizes = jnp.bincount(ids, length=N_EXPERTS)
    padded = (sizes + ROW_CHUNK - 1) // ROW_CHUNK * ROW_CHUNK
    pad_end = jnp.cumsum(padded)
    pad_start = pad_end - padded
    start = jnp.cumsum(sizes) - sizes
    dest = pad_start[sorted_ids] + jnp.arange(n_assign) - start[sorted_ids]
    n_chunks = -(-n_assign // ROW_CHUNK) + N_EXPERTS
    rows = jnp.zeros((n_chunks * ROW_CHUNK, d), h.dtype).at[dest].set(h[tok])
    chunk_expert = jnp.minimum(
        jnp.searchsorted(pad_end, jnp.arange(n_chunks) * ROW_CHUNK, side="right"), N_EXPERTS - 1)

    def expert_chunk(args):
        xc, e = args
        gate, up = jnp.split(jnp.matmul(xc, w_up[e]), 2, axis=-1)
        return jnp.matmul(jax.nn.silu(gate) * up, w_down[e])

    y_rows = lax.map(expert_chunk, (rows.reshape(n_chunks, ROW_CHUNK, d), chunk_expert))
    y = y_rows.reshape(-1, d)[dest] * weights[order][:, None].astype(h.dtype)
    out = jnp.zeros((t, d), h.dtype).at[tok].add(y)
    return out.reshape(b, s, d)


def setup_inputs(seed: int = 0) -> dict:
    key = jax.random.key(seed)
    ks = jax.random.split(key, 24)
    nrm = jax.random.normal
    f32 = jnp.float32
    x = nrm(ks[0], (BATCH, SEQ, D_MODEL), f32)
    even_w_in = nrm(ks[1], (N_EVEN, D_MODEL, D_IN_EVEN), f32) * D_MODEL ** -0.5
    even_conv_a = nrm(ks[2], (N_EVEN, CONV_A_WIDTH, D_CONV_A), f32) * CONV_A_WIDTH ** -0.5
    even_conv_b_w = nrm(ks[3], (N_EVEN, CONV_B_WIDTH, D_CONV_B), f32) * CONV_B_WIDTH ** -0.5
    even_conv_b_bias = nrm(ks[4], (N_EVEN, D_CONV_B), f32) * 0.02
    even_norm_b_g = 1.0 + 0.1 * nrm(ks[5], (N_EVEN, D_CONV_B), f32)
    even_norm_b_b = 0.02 * nrm(ks[6], (N_EVEN, D_CONV_B), f32)
    even_w_out = nrm(ks[7], (N_EVEN, D_MIX_EVEN, D_MODEL), f32) * (D_MIX_EVEN ** -0.5 * DEEPNORM_BETA)
    v_scale = jnp.concatenate([jnp.ones((2 * D_ATTN,), f32), jnp.full((D_ATTN,), DEEPNORM_BETA, f32)])
    odd_w_qkv = nrm(ks[8], (N_ODD, D_MODEL, 3 * D_ATTN), f32) * D_MODEL ** -0.5 * v_scale
    odd_w_o = nrm(ks[9], (N_ODD, D_ATTN, D_MODEL), f32) * (D_ATTN ** -0.5 * DEEPNORM_BETA)
    ln_mix_g = 1.0 + 0.1 * nrm(ks[10], (DEPTH, D_MODEL), f32)
    ln_mix_b = 0.02 * nrm(ks[11], (DEPTH, D_MODEL), f32)
    ln_ffn_g = 1.0 + 0.1 * nrm(ks[12], (DEPTH, D_MODEL), f32)
    ln_ffn_b = 0.02 * nrm(ks[13], (DEPTH, D_MODEL), f32)
    router_group_w = nrm(ks[14], (DEPTH, D_MODEL, N_GROUPS), f32) * D_MODEL ** -0.5
    router_group_b = 0.01 * nrm(ks[15], (DEPTH, N_GROUPS), f32)
    router_expert_w = nrm(ks[16], (DEPTH, D_MODEL, N_EXPERTS), f32) * D_MODEL ** -0.5
    router_expert_b = 0.01 * nrm(ks[17], (DEPTH, N_EXPERTS), f32)
    expert_w_up = nrm(ks[18], (DEPTH, N_EXPERTS, D_MODEL, 2 * D_EXPERT), f32) * D_MODEL ** -0.5
    expert_w_down = nrm(ks[19], (DEPTH, N_EXPERTS, D_EXPERT, D_MODEL), f32) * (D_EXPERT ** -0.5 * DEEPNORM_BETA)
    return {"x": x, "even_w_in": even_w_in, "even_conv_a": even_conv_a,
            "even_conv_b_w": even_conv_b_w, "even_conv_b_bias": even_conv_b_bias,
            "even_norm_b_g": even_norm_b_g, "even_norm_b_b": even_norm_b_b,
            "even_w_out": even_w_out, "odd_w_qkv": odd_w_qkv, "odd_w_o": odd_w_o,
            "ln_mix_g": ln_mix_g, "ln_mix_b": ln_mix_b, "ln_ffn_g": ln_ffn_g, "ln_ffn_b": ln_ffn_b,
            "router_group_w": router_group_w, "router_group_b": router_group_b,
            "router_expert_w": router_expert_w, "router_expert_b": router_expert_b,
            "expert_w_up": expert_w_up, "expert_w_down": expert_w_down}


def reference(x, even_w_in, even_conv_a, even_conv_b_w, even_conv_b_bias, even_norm_b_g,
              even_norm_b_b, even_w_out, odd_w_qkv, odd_w_o, ln_mix_g, ln_mix_b, ln_ffn_g,
              ln_ffn_b, router_group_w, router_group_b, router_expert_w, router_expert_b,
              expert_w_up, expert_w_down):
    for layer in range(DEPTH):
        i = layer // 2
        if layer % 2 == 0:
            mix = conv_mixers(x, even_w_in[i], even_conv_a[i], even_conv_b_w[i],
                              even_conv_b_bias[i], even_norm_b_g[i], even_norm_b_b[i],
                              even_w_out[i])
        else:
            mix = stick_breaking_attention(x, odd_w_qkv[i], odd_w_o[i])
        x = layer_norm(DEEPNORM_ALPHA * x + mix, ln_mix_g[layer], ln_mix_b[layer])
        ffn = hierarchical_moe(x, router_group_w[layer], router_group_b[layer],
                               router_expert_w[layer], router_expert_b[layer],
                               expert_w_up[layer], expert_w_down[layer])
        x = layer_norm(DEEPNORM_ALPHA * x + ffn, ln_ffn_g[layer], ln_ffn_b[layer])
    return x
```

```python
from contextlib import ExitStack
import numpy as np
import concourse.bass as bass
import concourse.mybir as mybir
from concourse.bass_utils import run_bass_kernel_spmd

F32 = mybir.dt.float32
BF16 = mybir.dt.bfloat16
ALU = mybir.AluOpType
ACT = mybir.ActivationFunctionType
AX = mybir.AxisListType

ENGS = ("tensor", "vector", "scalar", "gpsimd", "sync")
N_DMA_SLOTS = 8

D_MODEL = 1024
NCH = 8
DEPTH = 4
N_EXPERTS = 32
ALPHA = float((2 * DEPTH) ** 0.25)
LN_EPS = 1e-5
TB = 512
NEG = -1.0e30
SKIP_MOE = False
ATT_POOL = 'vector'
ATT_HP = 8


class Buf:
    __slots__ = ("name", "writer", "readers", "excl")

    def __init__(self, name="", excl=False):
        self.name = name
        self.writer = None
        self.readers = []
        self.excl = excl


class Op:
    __slots__ = ("eng", "fn", "deps", "dma", "needed", "sem", "val", "idx", "slot_prev", "epoch")

    def __init__(self, eng, fn, dma, epoch):
        self.eng = eng
        self.fn = fn
        self.dma = dma
        self.deps = []
        self.needed = False
        self.sem = None
        self.val = None
        self.idx = 0
        self.slot_prev = None
        self.epoch = epoch


class Prog:
    def __init__(self):
        self.ops = {e: [] for e in ENGS}
        self.epoch = 0
        self.dma_count = {e: 0 for e in ENGS}
        self.dma_last = {}
        self.all_bufs = []

    def buf(self, name="", excl=False):
        b = Buf(name, excl)
        self.all_bufs.append(b)
        return b

    def bufs(self, n, name=""):
        return [self.buf(f"{name}{i}") for i in range(n)]

    def add(self, eng, fn, reads=(), writes=(), dma=False, after=()):
        op = Op(eng, fn, dma, self.epoch)
        deps = {}

        def dep(o):
            if o is None or o is op:
                return
            if (not dma) and (not o.dma) and eng == "tensor" and o.eng == "tensor":
                return
            deps[id(o)] = o

        xr = [b for b in reads if b.excl and not (eng == "tensor" and not dma)]
        reads = [b for b in reads if b not in xr]
        writes = list(writes) + xr
        for b in reads:
            dep(b.writer)
        for b in writes:
            dep(b.writer)
            for r in b.readers:
                dep(r)
        for o in after:
            dep(o)
        op.deps = list(deps.values())
        for d in op.deps:
            d.needed = True
        for b in reads:
            if not dma:
                b.readers = [r for r in b.readers if r.dma or r.eng != eng]
            b.readers.append(op)
        for b in writes:
            b.writer = op
            b.readers = []
        if dma:
            n = self.dma_count[eng]
            self.dma_count[eng] = n + 1
            slot = n % N_DMA_SLOTS
            op.slot_prev = self.dma_last.get((eng, slot))
            self.dma_last[(eng, slot)] = op
            op.idx = n
        self.ops[eng].append(op)
        return op

    def barrier(self, new_epoch=False):
        lasts = []
        for e in ENGS:
            for o in reversed(self.ops[e]):
                if not o.dma:
                    lasts.append(o)
                    break
        dmas = list(self.dma_last.values())
        marks = [self.add(e, lambda eng: eng.nop(), after=lasts + dmas) for e in ENGS]
        for e in ENGS:
            self.add(e, lambda eng: eng.nop(), after=marks)
        for b in self.all_bufs:
            b.writer = None
            b.readers = []
        if new_epoch:
            self.epoch += 1

    def emit(self, block, esems, dsems):
        for e in ENGS:
            cnt = {}
            slot_uses = [0] * N_DMA_SLOTS
            for o in self.ops[e]:
                if o.dma:
                    s = o.idx % N_DMA_SLOTS
                    slot_uses[s] += 1
                    o.sem = dsems[e][s]
                    o.val = 16 * slot_uses[s]
                elif o.needed:
                    cnt[o.epoch] = cnt.get(o.epoch, 0) + 1
                    o.sem = esems[e][o.epoch]
                    o.val = cnt[o.epoch]

        def run(e):
            def body(eng):
                waited = {}
                for o in self.ops[e]:
                    need = {}
                    dl = list(o.deps)
                    if o.dma and o.slot_prev is not None:
                        dl.append(o.slot_prev)
                    for d in dl:
                        k = id(d.sem)
                        if k not in need or need[k][1] < d.val:
                            need[k] = (d.sem, d.val)
                    for k, (sem, val) in need.items():
                        if waited.get(k, 0) >= val:
                            continue
                        waited[k] = val
                        eng.wait_ge(sem, val)
                    ins = o.fn(eng)
                    if o.dma:
                        ins.then_inc(o.sem, 16)
                    elif o.needed:
                        ins.then_inc(o.sem, 1)
            return body

        block.tensor(run("tensor"))
        block.vector(run("vector"))
        block.scalar(run("scalar"))
        block.gpsimd(run("gpsimd"))
        block.sync(run("sync"))


class Carver:
    def __init__(self, pool_ap, total):
        self.ap = pool_ap
        self.total = total
        self.off = 0

    def f32(self, n):
        assert self.off + n <= self.total, ("SBUF pool overflow", self.off, n, self.total)
        v = self.ap[:, self.off:self.off + n]
        self.off += n
        return v

    def bf16(self, n):
        assert n % 2 == 0
        return self.f32(n // 2).bitcast(BF16)


def r3(ap, a):
    return ap.rearrange("p (a b) -> p a b", a=a)


def build_program(NSEQ, S, layers, dbg_after=None):
    assert S % TB == 0
    NT = S // TB
    NTL = S // 128
    nc = bass.Bass("TRN2", target_bir_lowering=False)

    def din(name, shape):
        return nc.dram_tensor(name, list(shape), F32, kind="ExternalInput").ap()

    xT = din("xT", [NSEQ, D_MODEL, S])
    yT = nc.dram_tensor("yT", [NSEQ, D_MODEL, S], F32, kind="ExternalOutput").ap()
    consts = din("consts", [128, 3 * 128 + 4 * TB])
    W = {}
    for l in layers:
        if l % 2 == 0:
            W[("win", l)] = din(f"win{l}", [D_MODEL, 2560])
            W[("wout", l)] = din(f"wout{l}", [D_MODEL, D_MODEL])
            W[("evp", l)] = din(f"evp{l}", [512, 37])
        else:
            W[("wqkv", l)] = din(f"wqkv{l}", [D_MODEL, 3072])
            W[("wo", l)] = din(f"wo{l}", [D_MODEL, D_MODEL])
        W[("lnp", l)] = din(f"lnp{l}", [D_MODEL, 4])
        W[("wr", l)] = din(f"wr{l}", [D_MODEL, 36])
        W[("br", l)] = din(f"br{l}", [1, 36])
        W[("wup", l)] = din(f"wup{l}", [N_EXPERTS, D_MODEL, 1024])
        W[("wdn", l)] = din(f"wdn{l}", [N_EXPERTS, 512, D_MODEL])

    P = Prog()
    with ExitStack() as es:
        POOLN = 53200
        pool_t = es.enter_context(nc.sbuf_tensor("pool", [128, POOLN], F32))
        psum = [es.enter_context(nc.psum_tensor(f"ps{i}", [128, TB], F32)) for i in range(8)]
        n_epochs = NSEQ + 1
        esems = {e: [es.enter_context(nc.semaphore(f"es_{e}{k}")) for k in range(n_epochs)] for e in ENGS}
        dsems = {e: [es.enter_context(nc.semaphore(f"ds_{e}{i}")) for i in range(N_DMA_SLOTS)] for e in ENGS}
        bps = [P.buf(f"ps{i}", excl=True) for i in range(8)]
        cv = Carver(pool_t, POOLN)

        xres = r3(cv.f32(NCH * S), NCH)
        xb = r3(cv.bf16(NCH * S), NCH)
        c_all = cv.f32(3 * 128)
        cU, cI, cOnes = c_all[:, 0:128], c_all[:, 128:256], c_all[:, 256:384]
        ones1024 = cv.bf16(128)
        ones512 = cv.bf16(128)
        b_x = P.bufs(NT, "x")
        b_xb = P.bufs(NT, "xb")
        b_const = P.buf("const")
        persist_off = cv.off

        P.add("sync", lambda e: e.dma_start(out=c_all, in_=consts[:, 0:384]), writes=[b_const], dma=True)
        P.add("gpsimd", lambda e: e.memset(ones1024, 1.0 / 1024.0), writes=[b_const])
        P.add("gpsimd", lambda e: e.memset(ones512, 1.0 / 512.0), writes=[b_const])

        def mm(out, lhsT, rhs, start, stop, reads, writes):
            return P.add("tensor", lambda e: e.matmul(out, lhsT, rhs, start=start, stop=stop),
                         reads=reads, writes=writes)

        def layer_norm(xv, bx, C, ones_bf, g_ap, b_ap, tmp, func, out_aps, out_bufs, bank_a, bank_b,
                       bf_out=None, bf_buf=None, lnb_off=0):
            lnb, b_lnb = tmp["lnb"], tmp["b_lnb"]
            m_sb, t2, rstd = tmp["m_sb"], tmp["t2"], tmp["rstd"]
            b_m, b_t2, b_rstd = tmp["b_m"], tmp["b_t2"], tmp["b_rstd"]
            lnb = lnb[:, lnb_off:lnb_off + C, :]
            lv = lnb
            pa, pb = psum[bank_a], psum[bank_b]
            P.add("scalar", lambda e: e.activation(out=lv, in_=xv, func=ACT.Copy), reads=[bx], writes=[b_lnb])
            for c in range(C):
                mm(pa[:], ones_bf, lnb[:, c, :], c == 0, c == C - 1, [b_lnb, b_const], [bps[bank_a]])
            P.add("scalar", lambda e: e.activation(out=lv, in_=xv, func=ACT.Square), reads=[bx], writes=[b_lnb])
            for c in range(C):
                mm(pb[:], ones_bf, lnb[:, c, :], c == 0, c == C - 1, [b_lnb, b_const], [bps[bank_b]])
            P.add("scalar", lambda e: e.activation(out=m_sb, in_=pa[:], func=ACT.Copy), reads=[bps[bank_a]], writes=[b_m])
            P.add("vector", lambda e: e.tensor_tensor(t2, m_sb, m_sb, op=ALU.mult), reads=[b_m], writes=[b_t2])
            P.add("vector", lambda e: e.tensor_tensor(t2, pb[:], t2, op=ALU.subtract), reads=[bps[bank_b], b_t2], writes=[b_t2])
            P.add("scalar", lambda e: e.activation(out=rstd, in_=t2, func=ACT.Ln, bias=LN_EPS), reads=[b_t2], writes=[b_rstd])
            P.add("scalar", lambda e: e.activation(out=rstd, in_=rstd, func=ACT.Exp, scale=-0.5), reads=[b_rstd], writes=[b_rstd])
            P.add("vector", lambda e: e.tensor_tensor(xv, xv, m_sb.unsqueeze(1).to_broadcast([128, C, TB]), op=ALU.subtract),
                  reads=[bx, b_m], writes=[bx])
            P.add("vector", lambda e: e.tensor_tensor(xv, xv, rstd.unsqueeze(1).to_broadcast([128, C, TB]), op=ALU.mult),
                  reads=[bx, b_rstd], writes=[bx])
            for c in range(C):
                P.add("scalar", lambda e, c=c: e.activation(out=out_aps[c], in_=xv[:, c, :], func=func,
                                                             scale=g_ap[:, c:c + 1], bias=b_ap[:, c:c + 1]),
                      reads=[bx, b_const], writes=[out_bufs[c]])
            if bf_out is not None:
                P.add("gpsimd", lambda e: e.tensor_copy(bf_out, xv), reads=[bx], writes=[bf_buf])

        def ln_tmp():
            return dict(lnb=r3(cv.bf16(NCH * TB), NCH), b_lnb=P.buf("lnb"),
                        m_sb=cv.f32(TB), t2=cv.f32(TB), rstd=cv.f32(TB),
                        b_m=P.buf("m"), b_t2=P.buf("t2"), b_rstd=P.buf("rstd"))

        def load_lnp(l):
            lnp = r3(cv.f32(NCH * 4), NCH)
            P.add("sync", lambda e: e.dma_start(out=lnp, in_=W[("lnp", l)].rearrange("(c p) k -> p c k", p=128)),
                  writes=[b_const], dma=True)
            return lnp

        def residual_ln(tb, lnp, gi, tmp, banks):
            xv = xres[:, :, tb * TB:(tb + 1) * TB]
            layer_norm(xv, b_x[tb], NCH, ones1024, lnp[:, :, gi], lnp[:, :, gi + 1], tmp, ACT.Identity,
                       [xv[:, c, :] for c in range(NCH)], [b_x[tb]] * NCH, banks[0], banks[1],
                       bf_out=xb[:, :, tb * TB:(tb + 1) * TB], bf_buf=b_xb[tb])

        def even_stage(l):
            cv.off = persist_off
            win = r3(cv.bf16(NCH * 2560), NCH)
            wout = r3(cv.bf16(NCH * 1024), NCH)
            evp = r3(cv.f32(4 * 37), 4)
            lnp = load_lnp(l)
            cvb = r3(cv.f32(4 * (TB + 2)), 4)
            gb = r3(cv.f32(4 * (TB + 30)), 4)
            gc = r3(cv.f32(4 * TB), 4)
            tmpA = [cv.f32(TB) for _ in range(2)]
            ya = [cv.f32(TB) for _ in range(2)]
            tmp = ln_tmp()
            ybuf = tmp["lnb"]
            b_y = tmp["b_lnb"]
            b_win, b_wout = P.buf("win"), P.buf("wout")
            b_cv, b_g, b_gc = P.bufs(4, "cv"), P.bufs(4, "g"), P.buf("gc")
            b_tmpA, b_ya = P.bufs(2, "tmpA"), P.bufs(2, "ya")
            winv = W[("win", l)].rearrange("(c p) n -> p c n", p=128)
            for q in range(5):
                P.add("gpsimd", lambda e, q=q: e.dma_start(out=win[:, :, q * 512:(q + 1) * 512], in_=winv[:, :, q * 512:(q + 1) * 512]),
                      writes=[b_win], dma=True)
            P.add("gpsimd", lambda e: e.dma_start(out=wout, in_=W[("wout", l)].rearrange("(c p) n -> p c n", p=128)),
                  writes=[b_wout], dma=True)
            P.add("sync", lambda e: e.dma_start(out=evp, in_=W[("evp", l)].rearrange("(c p) k -> p c k", p=128)),
                  writes=[b_const], dma=True)
            for j in range(4):
                P.add("gpsimd", lambda e, j=j: e.memset(cvb[:, j, 0:2], 0.0), writes=[b_cv[j]])
                P.add("gpsimd", lambda e, j=j: e.memset(gb[:, j, 0:30], 0.0), writes=[b_g[j]])
            cnt = [0]

            def proj(bank, col, tb):
                for c in range(NCH):
                    mm(psum[bank][:], win[:, c, col * 128:(col + 1) * 128], xb[:, c, tb * TB:(tb + 1) * TB],
                       c == 0, c == NCH - 1, [b_win, b_xb[tb]], [bps[bank]])

            for tb in range(NT):
                for j in range(4):
                    k = cnt[0] % 2
                    cnt[0] += 1
                    bA = (0, 1, 2) if k == 0 else (3, 4, 5)
                    proj(bA[0], 4 + j, tb)
                    proj(bA[1], 8 + j, tb)
                    proj(bA[2], j, tb)
                    P.add("scalar", lambda e, k=k, bA=bA: e.activation(out=tmpA[k], in_=psum[bA[0]][:], func=ACT.Copy),
                          reads=[bps[bA[0]]], writes=[b_tmpA[k]])
                    P.add("vector", lambda e, j=j, k=k, bA=bA: e.tensor_tensor(cvb[:, j, 2:2 + TB], tmpA[k], psum[bA[1]][:], op=ALU.mult),
                          reads=[b_tmpA[k], bps[bA[1]]], writes=[b_cv[j]])
                    P.add("vector", lambda e, j=j, k=k: e.tensor_scalar(ya[k], cvb[:, j, 0:TB], evp[:, j, 0:1], None, op0=ALU.mult),
                          reads=[b_cv[j], b_const], writes=[b_ya[k]])
                    for t in (1, 2):
                        P.add("vector", lambda e, j=j, k=k, t=t: e.scalar_tensor_tensor(ya[k], cvb[:, j, t:t + TB], evp[:, j, t:t + 1], ya[k],
                                                                                       op0=ALU.mult, op1=ALU.add),
                              reads=[b_cv[j], b_ya[k], b_const], writes=[b_ya[k]])
                    P.add("vector", lambda e, j=j, k=k, bA=bA: e.tensor_tensor(ybuf[:, j, :], ya[k], psum[bA[2]][:], op=ALU.mult),
                          reads=[b_ya[k], bps[bA[2]]], writes=[b_y])
                    P.add("gpsimd", lambda e, j=j: e.tensor_copy(cvb[:, j, 0:2], cvb[:, j, TB:TB + 2]), reads=[b_cv[j]], writes=[b_cv[j]])
                for j in range(4):
                    k = cnt[0] % 2
                    cnt[0] += 1
                    bB = (6, 7) if k == 0 else (0, 1)
                    proj(bB[0], 12 + j, tb)
                    proj(bB[1], 16 + j, tb)
                    P.add("scalar", lambda e, k=k, bB=bB: e.activation(out=tmpA[k], in_=psum[bB[1]][:], func=ACT.Sigmoid),
                          reads=[bps[bB[1]]], writes=[b_tmpA[k]])
                    P.add("vector", lambda e, j=j, k=k, bB=bB: e.tensor_tensor(gb[:, j, 30:30 + TB], tmpA[k], psum[bB[0]][:], op=ALU.mult),
                          reads=[b_tmpA[k], bps[bB[0]]], writes=[b_g[j]])
                    ceng = "vector"
                    P.add(ceng, lambda e, j=j: e.tensor_scalar(gc[:, j, :], gb[:, j, 0:TB], evp[:, j, 3:4], evp[:, j, 34:35],
                                                               op0=ALU.mult, op1=ALU.add),
                          reads=[b_g[j], b_const], writes=[b_gc])
                    for t in range(1, 31):
                        P.add(ceng, lambda e, j=j, t=t: e.scalar_tensor_tensor(gc[:, j, :], gb[:, j, t:t + TB], evp[:, j, 3 + t:4 + t], gc[:, j, :],
                                                                               op0=ALU.mult, op1=ALU.add),
                              reads=[b_g[j], b_gc, b_const], writes=[b_gc])
                    P.add("gpsimd", lambda e, j=j: e.tensor_copy(gb[:, j, 0:30], gb[:, j, TB:TB + 30]), reads=[b_g[j]], writes=[b_g[j]])
                layer_norm(gc, b_gc, 4, ones512, evp[:, :, 35], evp[:, :, 36], tmp, ACT.Silu,
                           [ybuf[:, 4 + c, :] for c in range(4)], [b_y] * 4, 2, 3, lnb_off=4)
                for n in range(NCH):
                    bank = 4 + (n % 2)
                    for c in range(NCH):
                        mm(psum[bank][:], wout[:, c, n * 128:(n + 1) * 128], ybuf[:, c, :], c == 0, c == NCH - 1,
                           [b_wout, b_y], [bps[bank]])
                    xs = xres[:, n, tb * TB:(tb + 1) * TB]
                    P.add("vector", lambda e, xs=xs, bank=bank: e.scalar_tensor_tensor(xs, xs, ALPHA, psum[bank][:], op0=ALU.mult, op1=ALU.add),
                          reads=[b_x[tb], bps[bank]], writes=[b_x[tb]])
                residual_ln(tb, lnp, 0, tmp, (6, 7))

        def odd_stage(l):
            cv.off = persist_off
            lnp = load_lnp(l)
            obuf = r3(cv.bf16(NCH * S), NCH)
            q_sb = cv.bf16(S)
            kz = r3(cv.bf16(2 * S), 2)
            vz = cv.bf16(NTL * 2 * 128).rearrange("p (a h n) -> p a h n", a=NTL, h=2)
            wsl = [r3(cv.bf16(NCH * 128), NCH) for _ in range(3)]
            wo = r3(cv.bf16(NCH * 1024), NCH)
            masks = r3(cv.bf16(4 * TB), 4)
            NR = 2
            e_sb = [cv.f32(TB) for _ in range(NR)]
            sp_sb = [cv.f32(TB) for _ in range(NR)]
            z_one = cv.f32(TB)
            z_sb = [z_one for _ in range(NR)]
            ne_sb = e_sb
            a_sb = [cv.bf16(TB) for _ in range(NR)]
            ssum = [cv.f32(TB) for _ in range(2)]
            tmp = ln_tmp()
            b_obuf = P.bufs(NT, "obuf")
            b_q, b_k, b_v = P.buf("q"), P.buf("k"), P.buf("v")
            b_wsl = P.bufs(3, "wsl")
            b_wo, b_mask = P.buf("wo"), P.buf("mask")
            b_e, b_sp, b_a = P.bufs(NR, "e"), P.bufs(NR, "sp"), P.bufs(NR, "a")
            b_zone = P.buf("z")
            b_z = [b_zone] * NR
            b_ne = b_e
            b_ss = P.bufs(2, "ss")
            wv = W[("wqkv", l)].rearrange("(c p) n -> p c n", p=128)
            P.add("gpsimd", lambda e: e.dma_start(out=wo, in_=W[("wo", l)].rearrange("(c p) n -> p c n", p=128)),
                  writes=[b_wo], dma=True)
            P.add("gpsimd", lambda e: e.dma_start(out=masks, in_=consts[:, 384:384 + 4 * TB].rearrange("p (a b) -> p a b", a=4)),
                  writes=[b_mask], dma=True)
            P.add("gpsimd", lambda e: e.memset(kz, 0.0), writes=[b_k])
            P.add("gpsimd", lambda e: e.memset(vz, 0.0), writes=[b_v])
            ucnt = [0]
            for hp in range(ATT_HP):
                for i3 in range(3):
                    col = i3 * 1024 + hp * 128
                    P.add("gpsimd", lambda e, i3=i3, col=col: e.dma_start(out=wsl[i3], in_=wv[:, :, col:col + 128]),
                          writes=[b_wsl[i3]], dma=True)
                for tb in range(NT):
                    ts = slice(tb * TB, (tb + 1) * TB)
                    for c in range(NCH):
                        mm(psum[0][:], wsl[0][:, c, :], xb[:, c, ts], c == 0, c == NCH - 1, [b_wsl[0], b_xb[tb]], [bps[0]])
                    P.add("scalar", lambda e, ts=ts: e.activation(out=q_sb[:, ts], in_=psum[0][:], func=ACT.Copy, scale=0.125),
                          reads=[bps[0]], writes=[b_q])
                    for c in range(NCH):
                        mm(psum[1][:], wsl[1][:, c, :], xb[:, c, ts], c == 0, c == NCH - 1, [b_wsl[1], b_xb[tb]], [bps[1]])
                    P.add("scalar", lambda e, ts=ts: e.activation(out=kz[0:64, 0, ts], in_=psum[1][0:64, :], func=ACT.Copy),
                          reads=[bps[1]], writes=[b_k])
                    P.add("scalar", lambda e, ts=ts: e.activation(out=kz[64:128, 1, ts], in_=psum[1][64:128, :], func=ACT.Copy),
                          reads=[bps[1]], writes=[b_k])
                for g4 in range(NTL // 4):
                    bank = 2
                    for k4 in range(4):
                        tl = g4 * 4 + k4
                        for c in range(NCH):
                            mm(psum[bank][:, k4 * 128:(k4 + 1) * 128], xb[:, c, tl * 128:(tl + 1) * 128], wsl[2][:, c, :],
                               c == 0, c == NCH - 1, [b_wsl[2], b_xb[tl // 4]], [bps[bank]])
                    pv = psum[bank][:].rearrange("p (a n) -> p a n", a=4)
                    P.add("vector", lambda e, g4=g4, pv=pv: e.tensor_copy(vz[:, g4 * 4:(g4 + 1) * 4, 0, 0:64], pv[:, :, 0:64]),
                          reads=[bps[bank]], writes=[b_v])
                    P.add("vector", lambda e, g4=g4, pv=pv: e.tensor_copy(vz[:, g4 * 4:(g4 + 1) * 4, 1, 64:128], pv[:, :, 64:128]),
                          reads=[bps[bank]], writes=[b_v])
                for Q in range(NT):
                    ob = 6 + (Q % 2)
                    qs = slice(Q * TB, (Q + 1) * TB)
                    nb = 4 * Q + 4
                    first = True
                    for h in range(2):
                        si = h
                        for bi, b in enumerate(range(nb - 1, -1, -1)):
                            u = ucnt[0] % NR
                            zb = ucnt[0] % 3
                            cb = 3 + (ucnt[0] % 3)
                            ucnt[0] += 1
                            r = b - 4 * Q
                            last = (h == 1 and b == 0)
                            mm(psum[zb][:], kz[:, h, b * 128:(b + 1) * 128], q_sb[:, qs], True, True, [b_k, b_q], [bps[zb]])
                            P.add("scalar", lambda e, u=u, zb=zb: e.activation(out=e_sb[u], in_=psum[zb][:], func=ACT.Exp),
                                  reads=[bps[zb]], writes=[b_e[u]])
                            P.add("scalar", lambda e, u=u: e.activation(out=sp_sb[u], in_=e_sb[u], func=ACT.Ln, bias=1.0),
                                  reads=[b_e[u]], writes=[b_sp[u]])
                            if r >= 0:
                                P.add(ATT_POOL, lambda e, u=u, r=r: e.tensor_tensor(sp_sb[u], sp_sb[u], masks[:, r, :], op=ALU.mult),
                                      reads=[b_sp[u], b_mask], writes=[b_sp[u]])
                            mm(psum[cb][:], cU, sp_sb[u], True, bi == 0, [b_sp[u], b_const], [bps[cb]])
                            if bi > 0:
                                mm(psum[cb][:], cOnes, ssum[si], False, True, [b_ss[si], b_const], [bps[cb]])
                            P.add("vector", lambda e, u=u, zb=zb: e.tensor_copy(z_sb[u], psum[zb][:]), reads=[bps[zb]], writes=[b_z[u]])
                            P.add("vector", lambda e, u=u, cb=cb: e.tensor_tensor(ne_sb[u], psum[cb][:], z_sb[u], op=ALU.subtract),
                                  reads=[bps[cb], b_z[u]], writes=[b_ne[u]])
                            if b > 0:
                                if bi == 0:
                                    P.add(ATT_POOL, lambda e, u=u, si=si: e.tensor_copy(ssum[si], sp_sb[u]), reads=[b_sp[u]], writes=[b_ss[si]])
                                else:
                                    P.add(ATT_POOL, lambda e, u=u, si=si: e.tensor_tensor(ssum[si], ssum[si], sp_sb[u], op=ALU.add),
                                          reads=[b_sp[u], b_ss[si]], writes=[b_ss[si]])
                            P.add("scalar", lambda e, u=u: e.activation(out=a_sb[u], in_=ne_sb[u], func=ACT.Exp, scale=-1.0),
                                  reads=[b_ne[u]], writes=[b_a[u]])
                            if r >= 0:
                                P.add(ATT_POOL, lambda e, u=u, r=r: e.tensor_tensor(a_sb[u], a_sb[u], masks[:, r, :], op=ALU.mult),
                                      reads=[b_a[u], b_mask], writes=[b_a[u]])
                            mm(psum[ob][:], vz[:, b, h, :], a_sb[u], first, last, [b_v, b_a[u]], [bps[ob]])
                            first = False
                    P.add("vector", lambda e, hp=hp, qs=qs, ob=ob: e.tensor_copy(obuf[:, hp, qs], psum[ob][:]),
                          reads=[bps[ob]], writes=[b_obuf[Q]])
            for tb in range(NT):
                ts = slice(tb * TB, (tb + 1) * TB)
                for n in range(NCH):
                    bank = n % 2
                    for c in range(NCH):
                        mm(psum[bank][:], wo[:, c, n * 128:(n + 1) * 128], obuf[:, c, ts], c == 0, c == NCH - 1,
                           [b_wo, b_obuf[tb]], [bps[bank]])
                    xs = xres[:, n, ts]
                    P.add("vector", lambda e, xs=xs, bank=bank: e.scalar_tensor_tensor(xs, xs, ALPHA, psum[bank][:], op0=ALU.mult, op1=ALU.add),
                          reads=[b_x[tb], bps[bank]], writes=[b_x[tb]])
                residual_ln(tb, lnp, 0, tmp, (2, 3))

        def moe_stage(l):
            cv.off = persist_off
            lnp = load_lnp(l)
            wr = r3(cv.f32(NCH * 36), NCH)
            br = cv.f32(36)
            wu = [r3(cv.bf16(NCH * 1024), NCH) for _ in range(2)]
            wd = [r3(cv.bf16(4 * 1024), 4) for _ in range(2)]
            lg = r3(cv.f32(NTL * 36), NTL)
            sel = r3(cv.f32(NTL * 32), NTL)
            oh1 = r3(cv.f32(NTL * 32), NTL)
            oh2 = r3(cv.f32(NTL * 32), NTL)
            Wt = r3(cv.f32(NTL * 32), NTL)
            ohg = r3(cv.f32(NTL * 4), NTL)
            ge = r3(cv.f32(NTL * 4), NTL)
            sm = [cv.f32(NTL) for _ in range(8)]
            Dt = [r3(cv.f32(4 * 128), 4) for _ in range(2)]
            sg = [cv.f32(TB) for _ in range(2)]
            tt = [cv.f32(TB) for _ in range(2)]
            hb = [r3(cv.bf16(4 * TB), 4) for _ in range(2)]
            tmp = ln_tmp()
            b_wr, b_r = P.buf("wr"), P.buf("router")
            b_wu, b_wd = P.bufs(2, "wu"), P.bufs(2, "wd")
            b_Dt, b_sg, b_tt, b_h = P.bufs(2, "Dt"), P.bufs(2, "sg"), P.bufs(2, "tt"), P.bufs(2, "h")
            P.add("sync", lambda e: e.dma_start(out=wr, in_=W[("wr", l)].rearrange("(c p) n -> p c n", p=128)), writes=[b_wr], dma=True)
            P.add("sync", lambda e: e.dma_start(out=br, in_=W[("br", l)].partition_broadcast(128)), writes=[b_wr], dma=True)
            TPB = 8
            for g8 in range((NTL + TPB - 1) // TPB):
                bank = g8 % 2
                nt8 = min(TPB, NTL - g8 * TPB)
                for k8 in range(nt8):
                    tl = g8 * TPB + k8
                    for c in range(NCH):
                        mm(psum[bank][:, k8 * 36:(k8 + 1) * 36], xres[:, c, tl * 128:(tl + 1) * 128], wr[:, c, :],
                           c == 0, c == NCH - 1, [b_x[tl // 4], b_wr], [bps[bank]])
                pv = psum[bank][:, 0:nt8 * 36].rearrange("p (a n) -> p a n", a=nt8)
                P.add("vector", lambda e, g8=g8, nt8=nt8, pv=pv: e.tensor_tensor(lg[:, g8 * TPB:g8 * TPB + nt8, :], pv,
                                                                               br.unsqueeze(1).to_broadcast([128, nt8, 36]), op=ALU.add),
                      reads=[bps[bank], b_wr], writes=[b_r])
            gl = lg[:, :, 0:4]
            el = lg[:, :, 4:36]
            gmax, gsum, ggate, m1, m2, dd, w1, w2 = sm

            def V(fn):
                P.add("vector", fn, reads=[b_r], writes=[b_r])

            def bc(ap, n):
                return ap.unsqueeze(2).to_broadcast([128, NTL, n])

            V(lambda e: e.reduce_max(gmax, gl, axis=AX.X))
            V(lambda e: e.tensor_tensor(ohg, gl, bc(gmax, 4), op=ALU.is_equal))
            V(lambda e: e.tensor_tensor(ge, gl, bc(gmax, 4), op=ALU.subtract))
            P.add("scalar", lambda e: e.activation(out=ge, in_=ge, func=ACT.Exp), reads=[b_r], writes=[b_r])
            V(lambda e: e.reduce_sum(gsum, ge, axis=AX.X))
            V(lambda e: e.reciprocal(ggate, gsum))
            V(lambda e: e.tensor_scalar(ohg, ohg, 1.0, -NEG, op0=ALU.subtract, op1=ALU.mult))
            V(lambda e: e.tensor_tensor(sel.rearrange("p a (g k) -> p a g k", g=4), el.rearrange("p a (g k) -> p a g k", g=4),
                                        ohg.unsqueeze(3).to_broadcast([128, NTL, 4, 8]), op=ALU.add))
            V(lambda e: e.reduce_max(m1, sel, axis=AX.X))
            V(lambda e: e.tensor_tensor(oh1, sel, bc(m1, 32), op=ALU.is_equal))
            V(lambda e: e.scalar_tensor_tensor(sel, oh1, NEG, sel, op0=ALU.mult, op1=ALU.add))
            V(lambda e: e.reduce_max(m2, sel, axis=AX.X))
            V(lambda e: e.tensor_tensor(oh2, sel, bc(m2, 32), op=ALU.is_equal))
            V(lambda e: e.tensor_tensor(dd, m2, m1, op=ALU.subtract))
            P.add("scalar", lambda e: e.activation(out=dd, in_=dd, func=ACT.Exp), reads=[b_r], writes=[b_r])
            V(lambda e: e.tensor_scalar(w1, dd, 1.0, None, op0=ALU.add))
            V(lambda e: e.reciprocal(w1, w1))
            V(lambda e: e.tensor_tensor(w2, dd, w1, op=ALU.mult))
            V(lambda e: e.tensor_tensor(w1, w1, ggate, op=ALU.mult))
            V(lambda e: e.tensor_tensor(w2, w2, ggate, op=ALU.mult))
            V(lambda e: e.tensor_tensor(oh1, oh1, bc(w1, 32), op=ALU.mult))
            V(lambda e: e.tensor_tensor(oh2, oh2, bc(w2, 32), op=ALU.mult))
            V(lambda e: e.tensor_tensor(Wt, oh1, oh2, op=ALU.add))
            for tb in range(NT):
                xv = xres[:, :, tb * TB:(tb + 1) * TB]
                P.add("scalar", lambda e, xv=xv: e.mul(xv, xv, ALPHA), reads=[b_x[tb]], writes=[b_x[tb]])
            wupv = W[("wup", l)]
            wdnv = W[("wdn", l)]
            ucnt = [0]
            for ex in range(N_EXPERTS):
                ws = ex % 2
                P.add("gpsimd", lambda e, ex=ex, ws=ws: e.dma_start(out=wu[ws], in_=wupv[ex].rearrange("(c p) n -> p c n", p=128)),
                      writes=[b_wu[ws]], dma=True)
                P.add("gpsimd", lambda e, ex=ex, ws=ws: e.dma_start(out=wd[ws], in_=wdnv[ex].rearrange("(c p) n -> p c n", p=128)),
                      writes=[b_wd[ws]], dma=True)
                for tb in range(NT):
                    ts = slice(tb * TB, (tb + 1) * TB)
                    u = ucnt[0] % 2
                    ucnt[0] += 1
                    wbk = u
                    for k4 in range(4):
                        tl = tb * 4 + k4
                        P.add("gpsimd", lambda e, u=u, k4=k4, tl=tl, ex=ex: e.tensor_scalar(Dt[u][:, k4, :], cI, Wt[:, tl, ex:ex + 1], None, op0=ALU.mult),
                              reads=[b_r, b_const], writes=[b_Dt[u]])
                        mm(psum[wbk][:, k4 * 128:(k4 + 1) * 128], cOnes, Dt[u][:, k4, :], True, True, [b_Dt[u], b_const], [bps[wbk]])
                    for j in range(4):
                        gbk = 2 + 2 * (j % 2)
                        ubk = gbk + 1
                        for c in range(NCH):
                            mm(psum[gbk][:], wu[ws][:, c, j * 128:(j + 1) * 128], xb[:, c, ts], c == 0, c == NCH - 1,
                               [b_wu[ws], b_xb[tb]], [bps[gbk]])
                        for c in range(NCH):
                            mm(psum[ubk][:], wu[ws][:, c, 512 + j * 128:512 + (j + 1) * 128], xb[:, c, ts], c == 0, c == NCH - 1,
                               [b_wu[ws], b_xb[tb]], [bps[ubk]])
                        k = j % 2
                        P.add("scalar", lambda e, k=k, gbk=gbk: e.activation(out=sg[k], in_=psum[gbk][:], func=ACT.Silu),
                              reads=[bps[gbk]], writes=[b_sg[k]])
                        P.add("vector", lambda e, k=k, ubk=ubk: e.tensor_tensor(tt[k], sg[k], psum[ubk][:], op=ALU.mult),
                              reads=[b_sg[k], bps[ubk]], writes=[b_tt[k]])
                        P.add("vector", lambda e, k=k, u=u, j=j, wbk=wbk: e.tensor_tensor(hb[u][:, j, :], tt[k], psum[wbk][:], op=ALU.mult),
                              reads=[b_tt[k], bps[wbk]], writes=[b_h[u]])
                    for n in range(NCH):
                        ybk = 6 + (n % 2)
                        for j in range(4):
                            mm(psum[ybk][:], wd[ws][:, j, n * 128:(n + 1) * 128], hb[u][:, j, :], j == 0, j == 3,
                               [b_wd[ws], b_h[u]], [bps[ybk]])
                        xs = xres[:, n, ts]
                        P.add("vector", lambda e, xs=xs, ybk=ybk: e.tensor_tensor(xs, xs, psum[ybk][:], op=ALU.add),
                              reads=[b_x[tb], bps[ybk]], writes=[b_x[tb]])
            for tb in range(NT):
                residual_ln(tb, lnp, 2, tmp, (2, 3))

        for s in range(NSEQ):
            for c in range(NCH):
                P.add("sync", lambda e, s=s, c=c: e.dma_start(out=xres[:, c, :], in_=xT[s, c * 128:(c + 1) * 128, :]),
                      writes=b_x, dma=True)
            for tb in range(NT):
                ts = slice(tb * TB, (tb + 1) * TB)
                P.add("gpsimd", lambda e, ts=ts: e.tensor_copy(xb[:, :, ts], xres[:, :, ts]), reads=[b_x[tb]], writes=[b_xb[tb]])
            for l in layers:
                if l % 2 == 0:
                    even_stage(l)
                else:
                    odd_stage(l)
                P.barrier()
                if not SKIP_MOE:
                    moe_stage(l)
                    P.barrier()
            for c in range(NCH):
                P.add("sync", lambda e, s=s, c=c: e.dma_start(out=yT[s, c * 128:(c + 1) * 128, :], in_=xres[:, c, :]),
                      reads=b_x, writes=[], dma=True)
            P.barrier(new_epoch=True)
        with nc.Block() as block:
            P.emit(block, esems, dsems)
    return nc


def make_consts():
    j = np.arange(128)[:, None]
    s = np.arange(128)[None, :]
    U = (j >= s).astype(np.float32)
    I = np.eye(128, dtype=np.float32)
    ones = np.ones((128, 128), np.float32)
    t = np.arange(TB)[None, :]
    masks = [(t > (r * 128 + j)).astype(np.float32) for r in range(4)]
    return np.ascontiguousarray(np.concatenate([U, I, ones] + masks, axis=1))


def layer_inputs(inp, layers):
    f = np.float32
    d = {"consts": make_consts()}
    for l in layers:
        i = l // 2
        if l % 2 == 0:
            d[f"win{l}"] = np.ascontiguousarray(inp["even_w_in"][i], f)
            d[f"wout{l}"] = np.ascontiguousarray(inp["even_w_out"][i], f)
            d[f"evp{l}"] = np.ascontiguousarray(np.concatenate(
                [inp["even_conv_a"][i].T, inp["even_conv_b_w"][i].T, inp["even_conv_b_bias"][i][:, None],
                 inp["even_norm_b_g"][i][:, None], inp["even_norm_b_b"][i][:, None]], axis=1), f)
        else:
            d[f"wqkv{l}"] = np.ascontiguousarray(inp["odd_w_qkv"][i], f)
            d[f"wo{l}"] = np.ascontiguousarray(inp["odd_w_o"][i], f)
        d[f"lnp{l}"] = np.ascontiguousarray(np.stack(
            [inp["ln_mix_g"][l], inp["ln_mix_b"][l], inp["ln_ffn_g"][l], inp["ln_ffn_b"][l]], axis=1), f)
        d[f"wr{l}"] = np.ascontiguousarray(np.concatenate([inp["router_group_w"][l], inp["router_expert_w"][l]], axis=1), f)
        d[f"br{l}"] = np.ascontiguousarray(np.concatenate([inp["router_group_b"][l], inp["router_expert_b"][l]])[None, :], f)
        d[f"wup{l}"] = np.ascontiguousarray(inp["expert_w_up"][l], f)
        d[f"wdn{l}"] = np.ascontiguousarray(inp["expert_w_down"][l], f)
    return d


def run_layers(x, inp, layers, n_cores):
    B, S, _ = x.shape
    assert B % n_cores == 0
    nseq = B // n_cores
    nc = build_program(nseq, S, layers)
    shared = layer_inputs(inp, layers)
    xt = np.ascontiguousarray(np.transpose(np.asarray(x, np.float32), (0, 2, 1)))
    in_maps = []
    for c in range(n_cores):
        m = dict(shared)
        m["xT"] = xt[c * nseq:(c + 1) * nseq]
        in_maps.append(m)
    res = run_bass_kernel_spmd(nc, in_maps, core_ids=list(range(n_cores)))
    yt = np.concatenate([r["yT"] for r in res.results], axis=0)
    return np.ascontiguousarray(np.transpose(yt, (0, 2, 1)))


def kernel(**inputs):
    x = np.asarray(inputs["x"], np.float32)
    return run_layers(x, inputs, [0, 1, 2, 3], 8)
```

```python
from contextlib import ExitStack
import numpy as np
import concourse.bass as bass
import concourse.mybir as mybir
from concourse.bass_utils import run_bass_kernel_spmd

F32 = mybir.dt.float32
BF16 = mybir.dt.bfloat16
ALU = mybir.AluOpType
ACT = mybir.ActivationFunctionType
AX = mybir.AxisListType

ENGS = ("tensor", "vector", "scalar", "gpsimd", "sync")
N_DMA_SLOTS = 8

D_MODEL = 1024
NCH = 8
DEPTH = 4
N_EXPERTS = 32
ALPHA = float((2 * DEPTH) ** 0.25)
LN_EPS = 1e-5
TB = 512
NEG = -1.0e30
SKIP_MOE = False
ATT_POOL = 'gpsimd'
ATT_HP = 8


class Buf:
    __slots__ = ("name", "writer", "readers", "excl")

    def __init__(self, name="", excl=False):
        self.name = name
        self.writer = None
        self.readers = []
        self.excl = excl


class Op:
    __slots__ = ("eng", "fn", "deps", "dma", "needed", "sem", "val", "idx", "slot_prev", "epoch")

    def __init__(self, eng, fn, dma, epoch):
        self.eng = eng
        self.fn = fn
        self.dma = dma
        self.deps = []
        self.needed = False
        self.sem = None
        self.val = None
        self.idx = 0
        self.slot_prev = None
        self.epoch = epoch


class Prog:
    def __init__(self):
        self.ops = {e: [] for e in ENGS}
        self.epoch = 0
        self.dma_count = {e: 0 for e in ENGS}
        self.dma_last = {}
        self.all_bufs = []

    def buf(self, name="", excl=False):
        b = Buf(name, excl)
        self.all_bufs.append(b)
        return b

    def bufs(self, n, name=""):
        return [self.buf(f"{name}{i}") for i in range(n)]

    def add(self, eng, fn, reads=(), writes=(), dma=False, after=()):
        op = Op(eng, fn, dma, self.epoch)
        deps = {}

        def dep(o):
            if o is None or o is op:
                return
            if (not dma) and (not o.dma) and eng == "tensor" and o.eng == "tensor":
                return
            deps[id(o)] = o

        xr = [b for b in reads if b.excl and not (eng == "tensor" and not dma)]
        reads = [b for b in reads if b not in xr]
        writes = list(writes) + xr
        for b in reads:
            dep(b.writer)
        for b in writes:
            dep(b.writer)
            for r in b.readers:
                dep(r)
        for o in after:
            dep(o)
        op.deps = list(deps.values())
        for d in op.deps:
            d.needed = True
        for b in reads:
            if not dma:
                b.readers = [r for r in b.readers if r.dma or r.eng != eng]
            b.readers.append(op)
        for b in writes:
            b.writer = op
            b.readers = []
        if dma:
            n = self.dma_count[eng]
            self.dma_count[eng] = n + 1
            slot = n % N_DMA_SLOTS
            op.slot_prev = self.dma_last.get((eng, slot))
            self.dma_last[(eng, slot)] = op
            op.idx = n
        self.ops[eng].append(op)
        return op

    def barrier(self, new_epoch=False):
        lasts = []
        for e in ENGS:
            for o in reversed(self.ops[e]):
                if not o.dma:
                    lasts.append(o)
                    break
        dmas = list(self.dma_last.values())
        marks = [self.add(e, lambda eng: eng.nop(), after=lasts + dmas) for e in ENGS]
        for e in ENGS:
            self.add(e, lambda eng: eng.nop(), after=marks)
        for b in self.all_bufs:
            b.writer = None
            b.readers = []
        if new_epoch:
            self.epoch += 1

    def emit(self, block, esems, dsems):
        for e in ENGS:
            cnt = {}
            slot_uses = [0] * N_DMA_SLOTS
            for o in self.ops[e]:
                if o.dma:
                    s = o.idx % N_DMA_SLOTS
                    slot_uses[s] += 1
                    o.sem = dsems[e][s]
                    o.val = 16 * slot_uses[s]
                elif o.needed:
                    cnt[o.epoch] = cnt.get(o.epoch, 0) + 1
                    o.sem = esems[e][o.epoch]
                    o.val = cnt[o.epoch]

        def run(e):
            def body(eng):
                waited = {}
                for o in self.ops[e]:
                    need = {}
                    dl = list(o.deps)
                    if o.dma and o.slot_prev is not None:
                        dl.append(o.slot_prev)
                    for d in dl:
                        k = id(d.sem)
                        if k not in need or need[k][1] < d.val:
                            need[k] = (d.sem, d.val)
                    for k, (sem, val) in need.items():
                        if waited.get(k, 0) >= val:
                            continue
                        waited[k] = val
                        eng.wait_ge(sem, val)
                    ins = o.fn(eng)
                    if o.dma:
                        ins.then_inc(o.sem, 16)
                    elif o.needed:
                        ins.then_inc(o.sem, 1)
            return body

        block.tensor(run("tensor"))
        block.vector(run("vector"))
        block.scalar(run("scalar"))
        block.gpsimd(run("gpsimd"))
        block.sync(run("sync"))


class Carver:
    def __init__(self, pool_ap, total):
        self.ap = pool_ap
        self.total = total
        self.off = 0

    def f32(self, n):
        assert self.off + n <= self.total, ("SBUF pool overflow", self.off, n, self.total)
        v = self.ap[:, self.off:self.off + n]
        self.off += n
        return v

    def bf16(self, n):
        assert n % 2 == 0
        return self.f32(n // 2).bitcast(BF16)


def r3(ap, a):
    return ap.rearrange("p (a b) -> p a b", a=a)


def build_program(NSEQ, S, layers, dbg_after=None):
    assert S % TB == 0
    NT = S // TB
    NTL = S // 128
    nc = bass.Bass("TRN2", target_bir_lowering=False)

    def din(name, shape):
        return nc.dram_tensor(name, list(shape), F32, kind="ExternalInput").ap()

    xT = din("xT", [NSEQ, D_MODEL, S])
    yT = nc.dram_tensor("yT", [NSEQ, D_MODEL, S], F32, kind="ExternalOutput").ap()
    consts = din("consts", [128, 3 * 128 + 4 * TB])
    selc = din("selc", [32, 32 * 128])
    W = {}
    for l in layers:
        if l % 2 == 0:
            W[("win", l)] = din(f"win{l}", [D_MODEL, 2560])
            W[("wout", l)] = din(f"wout{l}", [D_MODEL, D_MODEL])
            W[("evp", l)] = din(f"evp{l}", [512, 37])
        else:
            W[("wqkv", l)] = din(f"wqkv{l}", [D_MODEL, 3072])
            W[("wo", l)] = din(f"wo{l}", [D_MODEL, D_MODEL])
        W[("lnp", l)] = din(f"lnp{l}", [D_MODEL, 4])
        W[("wr", l)] = din(f"wr{l}", [D_MODEL, 36])
        W[("br", l)] = din(f"br{l}", [1, 36])
        W[("wup", l)] = din(f"wup{l}", [N_EXPERTS, D_MODEL, 1024])
        W[("wdn", l)] = din(f"wdn{l}", [N_EXPERTS, 512, D_MODEL])

    P = Prog()
    with ExitStack() as es:
        POOLN = 53200
        pool_t = es.enter_context(nc.sbuf_tensor("pool", [128, POOLN], F32))
        psum = [es.enter_context(nc.psum_tensor(f"ps{i}", [128, TB], F32)) for i in range(8)]
        n_epochs = NSEQ + 1
        esems = {e: [es.enter_context(nc.semaphore(f"es_{e}{k}")) for k in range(n_epochs)] for e in ENGS}
        dsems = {e: [es.enter_context(nc.semaphore(f"ds_{e}{i}")) for i in range(N_DMA_SLOTS)] for e in ENGS}
        bps = [P.buf(f"ps{i}", excl=True) for i in range(8)]
        cv = Carver(pool_t, POOLN)

        xres = r3(cv.f32(NCH * S), NCH)
        xb = r3(cv.bf16(NCH * S), NCH)
        c_all = cv.f32(3 * 128)
        cU, cI, cOnes = c_all[:, 0:128], c_all[:, 128:256], c_all[:, 256:384]
        ones1024 = cv.bf16(128)
        ones512 = cv.bf16(128)
        b_x = P.bufs(NT, "x")
        b_xb = P.bufs(NT, "xb")
        b_const = P.buf("const")
        persist_off = cv.off

        P.add("sync", lambda e: e.dma_start(out=c_all, in_=consts[:, 0:384]), writes=[b_const], dma=True)
        P.add("gpsimd", lambda e: e.memset(ones1024, 1.0 / 1024.0), writes=[b_const])
        P.add("gpsimd", lambda e: e.memset(ones512, 1.0 / 512.0), writes=[b_const])

        def mm(out, lhsT, rhs, start, stop, reads, writes):
            return P.add("tensor", lambda e: e.matmul(out, lhsT, rhs, start=start, stop=stop),
                         reads=reads, writes=writes)

        def layer_norm(xv, bx, C, ones_bf, g_ap, b_ap, tmp, func, out_aps, out_bufs, bank_a, bank_b,
                       bf_out=None, bf_buf=None, lnb_off=0):
            lnb, b_lnb = tmp["lnb"], tmp["b_lnb"]
            m_sb, t2, rstd = tmp["m_sb"], tmp["t2"], tmp["rstd"]
            b_m, b_t2, b_rstd = tmp["b_m"], tmp["b_t2"], tmp["b_rstd"]
            lnb = lnb[:, lnb_off:lnb_off + C, :]
            lv = lnb
            pa, pb = psum[bank_a], psum[bank_b]
            P.add("scalar", lambda e: e.activation(out=lv, in_=xv, func=ACT.Copy), reads=[bx], writes=[b_lnb])
            for c in range(C):
                mm(pa[:], ones_bf, lnb[:, c, :], c == 0, c == C - 1, [b_lnb, b_const], [bps[bank_a]])
            P.add("scalar", lambda e: e.activation(out=lv, in_=xv, func=ACT.Square), reads=[bx], writes=[b_lnb])
            for c in range(C):
                mm(pb[:], ones_bf, lnb[:, c, :], c == 0, c == C - 1, [b_lnb, b_const], [bps[bank_b]])
            P.add("scalar", lambda e: e.activation(out=m_sb, in_=pa[:], func=ACT.Copy), reads=[bps[bank_a]], writes=[b_m])
            P.add("vector", lambda e: e.tensor_tensor(t2, m_sb, m_sb, op=ALU.mult), reads=[b_m], writes=[b_t2])
            P.add("vector", lambda e: e.tensor_tensor(t2, pb[:], t2, op=ALU.subtract), reads=[bps[bank_b], b_t2], writes=[b_t2])
            P.add("scalar", lambda e: e.activation(out=rstd, in_=t2, func=ACT.Ln, bias=LN_EPS), reads=[b_t2], writes=[b_rstd])
            P.add("scalar", lambda e: e.activation(out=rstd, in_=rstd, func=ACT.Exp, scale=-0.5), reads=[b_rstd], writes=[b_rstd])
            P.add("vector", lambda e: e.tensor_tensor(xv, xv, m_sb.unsqueeze(1).to_broadcast([128, C, TB]), op=ALU.subtract),
                  reads=[bx, b_m], writes=[bx])
            P.add("vector", lambda e: e.tensor_tensor(xv, xv, rstd.unsqueeze(1).to_broadcast([128, C, TB]), op=ALU.mult),
                  reads=[bx, b_rstd], writes=[bx])
            for c in range(C):
                P.add("scalar", lambda e, c=c: e.activation(out=out_aps[c], in_=xv[:, c, :], func=func,
                                                             scale=g_ap[:, c:c + 1], bias=b_ap[:, c:c + 1]),
                      reads=[bx, b_const], writes=[out_bufs[c]])
            if bf_out is not None:
                P.add("gpsimd", lambda e: e.tensor_copy(bf_out, xv), reads=[bx], writes=[bf_buf])

        def ln_tmp():
            return dict(lnb=r3(cv.bf16(NCH * TB), NCH), b_lnb=P.buf("lnb"),
                        m_sb=cv.f32(TB), t2=cv.f32(TB), rstd=cv.f32(TB),
                        b_m=P.buf("m"), b_t2=P.buf("t2"), b_rstd=P.buf("rstd"))

        def load_lnp(l):
            lnp = r3(cv.f32(NCH * 4), NCH)
            P.add("sync", lambda e: e.dma_start(out=lnp, in_=W[("lnp", l)].rearrange("(c p) k -> p c k", p=128)),
                  writes=[b_const], dma=True)
            return lnp

        def residual_ln(tb, lnp, gi, tmp, banks):
            xv = xres[:, :, tb * TB:(tb + 1) * TB]
            layer_norm(xv, b_x[tb], NCH, ones1024, lnp[:, :, gi], lnp[:, :, gi + 1], tmp, ACT.Identity,
                       [xv[:, c, :] for c in range(NCH)], [b_x[tb]] * NCH, banks[0], banks[1],
                       bf_out=xb[:, :, tb * TB:(tb + 1) * TB], bf_buf=b_xb[tb])

        def even_stage(l):
            cv.off = persist_off
            win = r3(cv.bf16(NCH * 2560), NCH)
            wout = r3(cv.bf16(NCH * 1024), NCH)
            evp = r3(cv.f32(4 * 37), 4)
            lnp = load_lnp(l)
            cvb = r3(cv.f32(4 * (TB + 2)), 4)
            gb = r3(cv.f32(4 * (TB + 30)), 4)
            gc = r3(cv.f32(4 * TB), 4)
            tmpA = [cv.f32(TB) for _ in range(2)]
            ya = [cv.f32(TB) for _ in range(2)]
            tmp = ln_tmp()
            ybuf = tmp["lnb"]
            b_y = tmp["b_lnb"]
            b_win, b_wout = P.buf("win"), P.buf("wout")
            b_cv, b_g, b_gc = P.bufs(4, "cv"), P.bufs(4, "g"), P.buf("gc")
            b_tmpA, b_ya = P.bufs(2, "tmpA"), P.bufs(2, "ya")
            winv = W[("win", l)].rearrange("(c p) n -> p c n", p=128)
            for q in range(5):
                P.add("gpsimd", lambda e, q=q: e.dma_start(out=win[:, :, q * 512:(q + 1) * 512], in_=winv[:, :, q * 512:(q + 1) * 512]),
                      writes=[b_win], dma=True)
            P.add("gpsimd", lambda e: e.dma_start(out=wout, in_=W[("wout", l)].rearrange("(c p) n -> p c n", p=128)),
                  writes=[b_wout], dma=True)
            P.add("sync", lambda e: e.dma_start(out=evp, in_=W[("evp", l)].rearrange("(c p) k -> p c k", p=128)),
                  writes=[b_const], dma=True)
            for j in range(4):
                P.add("gpsimd", lambda e, j=j: e.memset(cvb[:, j, 0:2], 0.0), writes=[b_cv[j]])
                P.add("gpsimd", lambda e, j=j: e.memset(gb[:, j, 0:30], 0.0), writes=[b_g[j]])
            cnt = [0]

            def proj(bank, col, tb):
                for c in range(NCH):
                    mm(psum[bank][:], win[:, c, col * 128:(col + 1) * 128], xb[:, c, tb * TB:(tb + 1) * TB],
                       c == 0, c == NCH - 1, [b_win, b_xb[tb]], [bps[bank]])

            for tb in range(NT):
                for j in range(4):
                    k = cnt[0] % 2
                    cnt[0] += 1
                    bA = (0, 1, 2) if k == 0 else (3, 4, 5)
                    proj(bA[0], 4 + j, tb)
                    proj(bA[1], 8 + j, tb)
                    proj(bA[2], j, tb)
                    P.add("scalar", lambda e, k=k, bA=bA: e.activation(out=tmpA[k], in_=psum[bA[0]][:], func=ACT.Copy),
                          reads=[bps[bA[0]]], writes=[b_tmpA[k]])
                    P.add("vector", lambda e, j=j, k=k, bA=bA: e.tensor_tensor(cvb[:, j, 2:2 + TB], tmpA[k], psum[bA[1]][:], op=ALU.mult),
                          reads=[b_tmpA[k], bps[bA[1]]], writes=[b_cv[j]])
                    P.add("vector", lambda e, j=j, k=k: e.tensor_scalar(ya[k], cvb[:, j, 0:TB], evp[:, j, 0:1], None, op0=ALU.mult),
                          reads=[b_cv[j], b_const], writes=[b_ya[k]])
                    for t in (1, 2):
                        P.add("vector", lambda e, j=j, k=k, t=t: e.scalar_tensor_tensor(ya[k], cvb[:, j, t:t + TB], evp[:, j, t:t + 1], ya[k],
                                                                                       op0=ALU.mult, op1=ALU.add),
                              reads=[b_cv[j], b_ya[k], b_const], writes=[b_ya[k]])
                    P.add("vector", lambda e, j=j, k=k, bA=bA: e.tensor_tensor(ybuf[:, j, :], ya[k], psum[bA[2]][:], op=ALU.mult),
                          reads=[b_ya[k], bps[bA[2]]], writes=[b_y])
                    P.add("gpsimd", lambda e, j=j: e.tensor_copy(cvb[:, j, 0:2], cvb[:, j, TB:TB + 2]), reads=[b_cv[j]], writes=[b_cv[j]])
                for j in range(4):
                    k = cnt[0] % 2
                    cnt[0] += 1
                    bB = (6, 7) if k == 0 else (0, 1)
                    proj(bB[0], 12 + j, tb)
                    proj(bB[1], 16 + j, tb)
                    P.add("scalar", lambda e, k=k, bB=bB: e.activation(out=tmpA[k], in_=psum[bB[1]][:], func=ACT.Sigmoid),
                          reads=[bps[bB[1]]], writes=[b_tmpA[k]])
                    P.add("vector", lambda e, j=j, k=k, bB=bB: e.tensor_tensor(gb[:, j, 30:30 + TB], tmpA[k], psum[bB[0]][:], op=ALU.mult),
                          reads=[b_tmpA[k], bps[bB[0]]], writes=[b_g[j]])
                    ceng = "vector"
                    P.add(ceng, lambda e, j=j: e.tensor_scalar(gc[:, j, :], gb[:, j, 0:TB], evp[:, j, 3:4], evp[:, j, 34:35],
                                                               op0=ALU.mult, op1=ALU.add),
                          reads=[b_g[j], b_const], writes=[b_gc])
                    for t in range(1, 31):
                        P.add(ceng, lambda e, j=j, t=t: e.scalar_tensor_tensor(gc[:, j, :], gb[:, j, t:t + TB], evp[:, j, 3 + t:4 + t], gc[:, j, :],
                                                                               op0=ALU.mult, op1=ALU.add),
                              reads=[b_g[j], b_gc, b_const], writes=[b_gc])
                    P.add("gpsimd", lambda e, j=j: e.tensor_copy(gb[:, j, 0:30], gb[:, j, TB:TB + 30]), reads=[b_g[j]], writes=[b_g[j]])
                layer_norm(gc, b_gc, 4, ones512, evp[:, :, 35], evp[:, :, 36], tmp, ACT.Silu,
                           [ybuf[:, 4 + c, :] for c in range(4)], [b_y] * 4, 2, 3, lnb_off=4)
                for n in range(NCH):
                    bank = 4 + (n % 2)
                    for c in range(NCH):
                        mm(psum[bank][:], wout[:, c, n * 128:(n + 1) * 128], ybuf[:, c, :], c == 0, c == NCH - 1,
                           [b_wout, b_y], [bps[bank]])
                    xs = xres[:, n, tb * TB:(tb + 1) * TB]
                    P.add("vector", lambda e, xs=xs, bank=bank: e.scalar_tensor_tensor(xs, xs, ALPHA, psum[bank][:], op0=ALU.mult, op1=ALU.add),
                          reads=[b_x[tb], bps[bank]], writes=[b_x[tb]])
                residual_ln(tb, lnp, 0, tmp, (6, 7))

        def odd_stage(l):
            cv.off = persist_off
            lnp = load_lnp(l)
            obuf = r3(cv.bf16(NCH * S), NCH)
            q_sb = cv.bf16(S)
            kz = r3(cv.bf16(2 * S), 2)
            vz = cv.bf16(NTL * 2 * 128).rearrange("p (a h n) -> p a h n", a=NTL, h=2)
            wsl = [r3(cv.bf16(NCH * 128), NCH) for _ in range(3)]
            masks = r3(cv.bf16(4 * TB), 4)
            tmp = ln_tmp()
            ssum = [cv.f32(TB) for _ in range(2)]
            overlay_off = cv.off
            NR = 4
            e_sb = [cv.f32(TB) for _ in range(NR)]
            sp_sb = [cv.f32(TB) for _ in range(NR)]
            z_one = cv.f32(TB)
            z_sb = [z_one for _ in range(NR)]
            ne_sb = e_sb
            a_sb = [cv.bf16(TB) for _ in range(NR)]
            b_obuf = P.bufs(NT, "obuf")
            b_q, b_k, b_v = P.buf("q"), P.buf("k"), P.buf("v")
            b_wsl = P.bufs(3, "wsl")
            b_wo, b_mask = P.buf("wo"), P.buf("mask")
            b_e, b_sp, b_a = P.bufs(NR, "e"), P.bufs(NR, "sp"), P.bufs(NR, "a")
            b_zone = P.buf("z")
            b_z = [b_zone] * NR
            b_ne = b_e
            b_ss = P.bufs(2, "ss")
            wv = W[("wqkv", l)].rearrange("(c p) n -> p c n", p=128)
            P.add("gpsimd", lambda e: e.dma_start(out=masks, in_=consts[:, 384:384 + 4 * TB].rearrange("p (a b) -> p a b", a=4)),
                  writes=[b_mask], dma=True)
            P.add("gpsimd", lambda e: e.memset(kz, 0.0), writes=[b_k])
            P.add("gpsimd", lambda e: e.memset(vz, 0.0), writes=[b_v])
            ucnt = [0]
            for hp in range(ATT_HP):
                for i3 in range(3):
                    col = i3 * 1024 + hp * 128
                    P.add("gpsimd", lambda e, i3=i3, col=col: e.dma_start(out=wsl[i3], in_=wv[:, :, col:col + 128]),
                          writes=[b_wsl[i3]], dma=True)
                for tb in range(NT):
                    ts = slice(tb * TB, (tb + 1) * TB)
                    for c in range(NCH):
                        mm(psum[0][:], wsl[0][:, c, :], xb[:, c, ts], c == 0, c == NCH - 1, [b_wsl[0], b_xb[tb]], [bps[0]])
                    P.add("scalar", lambda e, ts=ts: e.activation(out=q_sb[:, ts], in_=psum[0][:], func=ACT.Copy, scale=0.125),
                          reads=[bps[0]], writes=[b_q])
                    for c in range(NCH):
                        mm(psum[1][:], wsl[1][:, c, :], xb[:, c, ts], c == 0, c == NCH - 1, [b_wsl[1], b_xb[tb]], [bps[1]])
                    P.add("scalar", lambda e, ts=ts: e.activation(out=kz[0:64, 0, ts], in_=psum[1][0:64, :], func=ACT.Copy),
                          reads=[bps[1]], writes=[b_k])
                    P.add("scalar", lambda e, ts=ts: e.activation(out=kz[64:128, 1, ts], in_=psum[1][64:128, :], func=ACT.Copy),
                          reads=[bps[1]], writes=[b_k])
                for g4 in range(NTL // 4):
                    bank = 2
                    for k4 in range(4):
                        tl = g4 * 4 + k4
                        for c in range(NCH):
                            mm(psum[bank][:, k4 * 128:(k4 + 1) * 128], xb[:, c, tl * 128:(tl + 1) * 128], wsl[2][:, c, :],
                               c == 0, c == NCH - 1, [b_wsl[2], b_xb[tl // 4]], [bps[bank]])
                    pv = psum[bank][:].rearrange("p (a n) -> p a n", a=4)
                    P.add("vector", lambda e, g4=g4, pv=pv: e.tensor_copy(vz[:, g4 * 4:(g4 + 1) * 4, 0, 0:64], pv[:, :, 0:64]),
                          reads=[bps[bank]], writes=[b_v])
                    P.add("vector", lambda e, g4=g4, pv=pv: e.tensor_copy(vz[:, g4 * 4:(g4 + 1) * 4, 1, 64:128], pv[:, :, 64:128]),
                          reads=[bps[bank]], writes=[b_v])
                for Q in range(NT):
                    ob = 6 + (Q % 2)
                    qs = slice(Q * TB, (Q + 1) * TB)
                    nb = 4 * Q + 4
                    ulist = [(h, b) for b in range(nb - 1, -1, -1) for h in range(2)]
                    st = {}

                    def phase_a(idx):
                        h, b = ulist[idx]
                        bi = (nb - 1) - b
                        u = ucnt[0] % NR
                        zb = ucnt[0] % 3
                        cb = 3 + (ucnt[0] % 3)
                        ucnt[0] += 1
                        st[idx] = u
                        r = b - 4 * Q
                        si = h
                        mm(psum[zb][:], kz[:, h, b * 128:(b + 1) * 128], q_sb[:, qs], True, True, [b_k, b_q], [bps[zb]])
                        P.add("scalar", lambda e: e.activation(out=e_sb[u], in_=psum[zb][:], func=ACT.Exp),
                              reads=[bps[zb]], writes=[b_e[u]])
                        P.add("scalar", lambda e: e.activation(out=sp_sb[u], in_=e_sb[u], func=ACT.Ln, bias=1.0),
                              reads=[b_e[u]], writes=[b_sp[u]])
                        if r >= 0:
                            P.add(ATT_POOL, lambda e: e.tensor_tensor(sp_sb[u], sp_sb[u], masks[:, r, :], op=ALU.mult),
                                  reads=[b_sp[u], b_mask], writes=[b_sp[u]])
                        mm(psum[cb][:], cU, sp_sb[u], True, bi == 0, [b_sp[u], b_const], [bps[cb]])
                        if bi > 0:
                            mm(psum[cb][:], cOnes, ssum[si], False, True, [b_ss[si], b_const], [bps[cb]])
                        P.add("vector", lambda e: e.tensor_copy(z_sb[u], psum[zb][:]), reads=[bps[zb]], writes=[b_z[u]])
                        P.add("vector", lambda e: e.tensor_tensor(ne_sb[u], psum[cb][:], z_sb[u], op=ALU.subtract),
                              reads=[bps[cb], b_z[u]], writes=[b_ne[u]])
                        if b > 0:
                            if bi == 0:
                                P.add(ATT_POOL, lambda e: e.tensor_copy(ssum[si], sp_sb[u]), reads=[b_sp[u]], writes=[b_ss[si]])
                            else:
                                P.add(ATT_POOL, lambda e: e.tensor_tensor(ssum[si], ssum[si], sp_sb[u], op=ALU.add),
                                      reads=[b_sp[u], b_ss[si]], writes=[b_ss[si]])

                    def phase_b(idx):
                        h, b = ulist[idx]
                        u = st[idx]
                        r = b - 4 * Q
                        P.add("scalar", lambda e: e.activation(out=a_sb[u], in_=ne_sb[u], func=ACT.Exp, scale=-1.0),
                              reads=[b_ne[u]], writes=[b_a[u]])
                        if r >= 0:
                            P.add(ATT_POOL, lambda e: e.tensor_tensor(a_sb[u], a_sb[u], masks[:, r, :], op=ALU.mult),
                                  reads=[b_a[u], b_mask], writes=[b_a[u]])
                        mm(psum[ob][:], vz[:, b, h, :], a_sb[u], idx == 0, idx == len(ulist) - 1, [b_v, b_a[u]], [bps[ob]])

                    SK = 2
                    for i in range(len(ulist) + SK):
                        if i < len(ulist):
                            phase_a(i)
                        if i - SK >= 0:
                            phase_b(i - SK)
                    P.add("vector", lambda e, hp=hp, qs=qs, ob=ob: e.tensor_copy(obuf[:, hp, qs], psum[ob][:]),
                          reads=[bps[ob]], writes=[b_obuf[Q]])
            P.barrier()
            cv.off = overlay_off
            wo = r3(cv.bf16(NCH * 1024), NCH)
            P.add("gpsimd", lambda e: e.dma_start(out=wo, in_=W[("wo", l)].rearrange("(c p) n -> p c n", p=128)),
                  writes=[b_wo], dma=True)
            for tb in range(NT):
                ts = slice(tb * TB, (tb + 1) * TB)
                for n in range(NCH):
                    bank = n % 2
                    for c in range(NCH):
                        mm(psum[bank][:], wo[:, c, n * 128:(n + 1) * 128], obuf[:, c, ts], c == 0, c == NCH - 1,
                           [b_wo, b_obuf[tb]], [bps[bank]])
                    xs = xres[:, n, ts]
                    P.add("vector", lambda e, xs=xs, bank=bank: e.scalar_tensor_tensor(xs, xs, ALPHA, psum[bank][:], op0=ALU.mult, op1=ALU.add),
                          reads=[b_x[tb], bps[bank]], writes=[b_x[tb]])
                residual_ln(tb, lnp, 0, tmp, (2, 3))

        def moe_stage(l):
            cv.off = persist_off
            lnp = load_lnp(l)
            wr = r3(cv.f32(NCH * 36), NCH)
            br = cv.f32(36)
            wu = [r3(cv.bf16(NCH * 1024), NCH) for _ in range(2)]
            wd = [r3(cv.bf16(4 * 1024), 4) for _ in range(2)]
            lg = r3(cv.f32(NTL * 36), NTL)
            sel = r3(cv.f32(NTL * 32), NTL)
            oh1 = r3(cv.f32(NTL * 32), NTL)
            oh2 = r3(cv.f32(NTL * 32), NTL)
            Wt = r3(cv.f32(NTL * 32), NTL)
            ohg = r3(cv.f32(NTL * 4), NTL)
            ge = r3(cv.f32(NTL * 4), NTL)
            sm = [cv.f32(NTL) for _ in range(8)]
            selb = r3(cv.bf16(32 * 128), 32)
            WThi = cv.bf16(S)
            WTlo = cv.bf16(S)
            sg = [cv.f32(TB) for _ in range(2)]
            tt = [cv.f32(TB) for _ in range(2)]
            hb = [r3(cv.bf16(4 * TB), 4) for _ in range(2)]
            tmp = ln_tmp()
            b_wr, b_r = P.buf("wr"), P.buf("router")
            b_wu, b_wd = P.bufs(2, "wu"), P.bufs(2, "wd")
            b_sg, b_tt, b_h = P.bufs(2, "sg"), P.bufs(2, "tt"), P.bufs(2, "h")
            b_wt, b_sel = P.buf("wt"), P.buf("sel")
            P.add("sync", lambda e: e.dma_start(out=wr, in_=W[("wr", l)].rearrange("(c p) n -> p c n", p=128)), writes=[b_wr], dma=True)
            P.add("sync", lambda e: e.dma_start(out=br, in_=W[("br", l)].partition_broadcast(128)), writes=[b_wr], dma=True)
            P.add("gpsimd", lambda e: e.dma_start(out=selb[0:32, :, :], in_=selc.rearrange("k (e m) -> k e m", e=32)), writes=[b_sel], dma=True)
            TPB = 8
            for g8 in range((NTL + TPB - 1) // TPB):
                bank = g8 % 2
                nt8 = min(TPB, NTL - g8 * TPB)
                for k8 in range(nt8):
                    tl = g8 * TPB + k8
                    for c in range(NCH):
                        mm(psum[bank][:, k8 * 36:(k8 + 1) * 36], xres[:, c, tl * 128:(tl + 1) * 128], wr[:, c, :],
                           c == 0, c == NCH - 1, [b_x[tl // 4], b_wr], [bps[bank]])
                pv = psum[bank][:, 0:nt8 * 36].rearrange("p (a n) -> p a n", a=nt8)
                P.add("vector", lambda e, g8=g8, nt8=nt8, pv=pv: e.tensor_tensor(lg[:, g8 * TPB:g8 * TPB + nt8, :], pv,
                                                                               br.unsqueeze(1).to_broadcast([128, nt8, 36]), op=ALU.add),
                      reads=[bps[bank], b_wr], writes=[b_r])
            gl = lg[:, :, 0:4]
            el = lg[:, :, 4:36]
            gmax, gsum, ggate, m1, m2, dd, w1, w2 = sm

            def V(fn):
                P.add("vector", fn, reads=[b_r], writes=[b_r])

            def bc(ap, n):
                return ap.unsqueeze(2).to_broadcast([128, NTL, n])

            V(lambda e: e.reduce_max(gmax, gl, axis=AX.X))
            V(lambda e: e.tensor_tensor(ohg, gl, bc(gmax, 4), op=ALU.is_equal))
            V(lambda e: e.tensor_tensor(ge, gl, bc(gmax, 4), op=ALU.subtract))
            P.add("scalar", lambda e: e.activation(out=ge, in_=ge, func=ACT.Exp), reads=[b_r], writes=[b_r])
            V(lambda e: e.reduce_sum(gsum, ge, axis=AX.X))
            V(lambda e: e.reciprocal(ggate, gsum))
            V(lambda e: e.tensor_scalar(ohg, ohg, 1.0, -NEG, op0=ALU.subtract, op1=ALU.mult))
            V(lambda e: e.tensor_tensor(sel.rearrange("p a (g k) -> p a g k", g=4), el.rearrange("p a (g k) -> p a g k", g=4),
                                        ohg.unsqueeze(3).to_broadcast([128, NTL, 4, 8]), op=ALU.add))
            V(lambda e: e.reduce_max(m1, sel, axis=AX.X))
            V(lambda e: e.tensor_tensor(oh1, sel, bc(m1, 32), op=ALU.is_equal))
            V(lambda e: e.scalar_tensor_tensor(sel, oh1, NEG, sel, op0=ALU.mult, op1=ALU.add))
            V(lambda e: e.reduce_max(m2, sel, axis=AX.X))
            V(lambda e: e.tensor_tensor(oh2, sel, bc(m2, 32), op=ALU.is_equal))
            V(lambda e: e.tensor_tensor(dd, m2, m1, op=ALU.subtract))
            P.add("scalar", lambda e: e.activation(out=dd, in_=dd, func=ACT.Exp), reads=[b_r], writes=[b_r])
            V(lambda e: e.tensor_scalar(w1, dd, 1.0, None, op0=ALU.add))
            V(lambda e: e.reciprocal(w1, w1))
            V(lambda e: e.tensor_tensor(w2, dd, w1, op=ALU.mult))
            V(lambda e: e.tensor_tensor(w1, w1, ggate, op=ALU.mult))
            V(lambda e: e.tensor_tensor(w2, w2, ggate, op=ALU.mult))
            V(lambda e: e.tensor_tensor(oh1, oh1, bc(w1, 32), op=ALU.mult))
            V(lambda e: e.tensor_tensor(oh2, oh2, bc(w2, 32), op=ALU.mult))
            V(lambda e: e.tensor_tensor(Wt, oh1, oh2, op=ALU.add))
            for tb in range(NT):
                xv = xres[:, :, tb * TB:(tb + 1) * TB]
                P.add("scalar", lambda e, xv=xv: e.mul(xv, xv, ALPHA), reads=[b_x[tb]], writes=[b_x[tb]])
            for tb in range(NT):
                ts = slice(tb * TB, (tb + 1) * TB)
                bank = 2 + (tb % 2)
                for k4 in range(4):
                    tl = tb * 4 + k4
                    mm(psum[bank][0:32, k4 * 128:(k4 + 1) * 128], Wt[:, tl, :], cI, True, True, [b_r, b_const], [bps[bank]])
                P.add("scalar", lambda e, ts=ts, bank=bank: e.activation(out=WThi[0:32, ts], in_=psum[bank][0:32, :], func=ACT.Copy),
                      reads=[bps[bank]], writes=[b_wt])
                P.add("vector", lambda e, ts=ts, bank=bank: e.tensor_tensor(WTlo[0:32, ts], psum[bank][0:32, :], WThi[0:32, ts], op=ALU.subtract),
                      reads=[bps[bank], b_wt], writes=[b_wt])
            wupv = W[("wup", l)]
            wdnv = W[("wdn", l)]
            items = [(ex, tb) for ex in range(N_EXPERTS) for tb in range(NT)]

            def stage_up(i):
                ex, tb = items[i]
                ws = ex % 2
                u = i % 2
                ts = slice(tb * TB, (tb + 1) * TB)
                if tb == 0:
                    P.add("gpsimd", lambda e: e.dma_start(out=wu[ws], in_=wupv[ex].rearrange("(c p) n -> p c n", p=128)),
                          writes=[b_wu[ws]], dma=True)
                    P.add("gpsimd", lambda e: e.dma_start(out=wd[ws], in_=wdnv[ex].rearrange("(c p) n -> p c n", p=128)),
                          writes=[b_wd[ws]], dma=True)
                wbk = u
                mm(psum[wbk][:], selb[0:32, ex, :], WThi[0:32, ts], True, False, [b_wt, b_sel], [bps[wbk]])
                mm(psum[wbk][:], selb[0:32, ex, :], WTlo[0:32, ts], False, True, [b_wt, b_sel], [bps[wbk]])
                for j in range(4):
                    gbk = 2 + 2 * (j % 2)
                    ubk = gbk + 1
                    for c in range(NCH):
                        mm(psum[gbk][:], wu[ws][:, c, j * 128:(j + 1) * 128], xb[:, c, ts], c == 0, c == NCH - 1,
                           [b_wu[ws], b_xb[tb]], [bps[gbk]])
                    for c in range(NCH):
                        mm(psum[ubk][:], wu[ws][:, c, 512 + j * 128:512 + (j + 1) * 128], xb[:, c, ts], c == 0, c == NCH - 1,
                           [b_wu[ws], b_xb[tb]], [bps[ubk]])
                    k = j % 2
                    P.add("scalar", lambda e, k=k, gbk=gbk: e.activation(out=sg[k], in_=psum[gbk][:], func=ACT.Silu),
                          reads=[bps[gbk]], writes=[b_sg[k]])
                    P.add("vector", lambda e, k=k, ubk=ubk: e.tensor_tensor(tt[k], sg[k], psum[ubk][:], op=ALU.mult),
                          reads=[b_sg[k], bps[ubk]], writes=[b_tt[k]])
                    P.add("vector", lambda e, k=k, j=j: e.tensor_tensor(hb[u][:, j, :], tt[k], psum[wbk][:], op=ALU.mult),
                          reads=[b_tt[k], bps[wbk]], writes=[b_h[u]])

            def stage_down(i):
                ex, tb = items[i]
                ws = ex % 2
                u = i % 2
                ts = slice(tb * TB, (tb + 1) * TB)
                for n in range(NCH):
                    ybk = 6 + (n % 2)
                    for j in range(4):
                        mm(psum[ybk][:], wd[ws][:, j, n * 128:(n + 1) * 128], hb[u][:, j, :], j == 0, j == 3,
                           [b_wd[ws], b_h[u]], [bps[ybk]])
                    xs = xres[:, n, ts]
                    P.add("vector", lambda e, xs=xs, ybk=ybk: e.tensor_tensor(xs, xs, psum[ybk][:], op=ALU.add),
                          reads=[b_x[tb], bps[ybk]], writes=[b_x[tb]])

            stage_up(0)
            for i in range(len(items)):
                if i + 1 < len(items):
                    stage_up(i + 1)
                stage_down(i)
            for tb in range(NT):
                residual_ln(tb, lnp, 2, tmp, (2, 3))

        for s in range(NSEQ):
            for c in range(NCH):
                P.add("sync", lambda e, s=s, c=c: e.dma_start(out=xres[:, c, :], in_=xT[s, c * 128:(c + 1) * 128, :]),
                      writes=b_x, dma=True)
            for tb in range(NT):
                ts = slice(tb * TB, (tb + 1) * TB)
                P.add("gpsimd", lambda e, ts=ts: e.tensor_copy(xb[:, :, ts], xres[:, :, ts]), reads=[b_x[tb]], writes=[b_xb[tb]])
            for l in layers:
                if l % 2 == 0:
                    even_stage(l)
                else:
                    odd_stage(l)
                P.barrier()
                if not SKIP_MOE:
                    moe_stage(l)
                    P.barrier()
            for c in range(NCH):
                P.add("sync", lambda e, s=s, c=c: e.dma_start(out=yT[s, c * 128:(c + 1) * 128, :], in_=xres[:, c, :]),
                      reads=b_x, writes=[], dma=True)
            P.barrier(new_epoch=True)
        with nc.Block() as block:
            P.emit(block, esems, dsems)
    return nc


def make_consts():
    j = np.arange(128)[:, None]
    s = np.arange(128)[None, :]
    U = (j >= s).astype(np.float32)
    I = np.eye(128, dtype=np.float32)
    ones = np.ones((128, 128), np.float32)
    t = np.arange(TB)[None, :]
    masks = [(t > (r * 128 + j)).astype(np.float32) for r in range(4)]
    return np.ascontiguousarray(np.concatenate([U, I, ones] + masks, axis=1))


def layer_inputs(inp, layers):
    f = np.float32
    d = {"consts": make_consts(),
         "selc": np.ascontiguousarray(np.repeat(np.eye(32, dtype=np.float32), 128, axis=1))}
    for l in layers:
        i = l // 2
        if l % 2 == 0:
            d[f"win{l}"] = np.ascontiguousarray(inp["even_w_in"][i], f)
            d[f"wout{l}"] = np.ascontiguousarray(inp["even_w_out"][i], f)
            d[f"evp{l}"] = np.ascontiguousarray(np.concatenate(
                [inp["even_conv_a"][i].T, inp["even_conv_b_w"][i].T, inp["even_conv_b_bias"][i][:, None],
                 inp["even_norm_b_g"][i][:, None], inp["even_norm_b_b"][i][:, None]], axis=1), f)
        else:
            d[f"wqkv{l}"] = np.ascontiguousarray(inp["odd_w_qkv"][i], f)
            d[f"wo{l}"] = np.ascontiguousarray(inp["odd_w_o"][i], f)
        d[f"lnp{l}"] = np.ascontiguousarray(np.stack(
            [inp["ln_mix_g"][l], inp["ln_mix_b"][l], inp["ln_ffn_g"][l], inp["ln_ffn_b"][l]], axis=1), f)
        d[f"wr{l}"] = np.ascontiguousarray(np.concatenate([inp["router_group_w"][l], inp["router_expert_w"][l]], axis=1), f)
        d[f"br{l}"] = np.ascontiguousarray(np.concatenate([inp["router_group_b"][l], inp["router_expert_b"][l]])[None, :], f)
        d[f"wup{l}"] = np.ascontiguousarray(inp["expert_w_up"][l], f)
        d[f"wdn{l}"] = np.ascontiguousarray(inp["expert_w_down"][l], f)
    return d


def run_layers(x, inp, layers, n_cores):
    B, S, _ = x.shape
    assert B % n_cores == 0
    nseq = B // n_cores
    nc = build_program(nseq, S, layers)
    shared = layer_inputs(inp, layers)
    xt = np.ascontiguousarray(np.transpose(np.asarray(x, np.float32), (0, 2, 1)))
    in_maps = []
    for c in range(n_cores):
        m = dict(shared)
        m["xT"] = xt[c * nseq:(c + 1) * nseq]
        in_maps.append(m)
    res = run_bass_kernel_spmd(nc, in_maps, core_ids=list(range(n_cores)))
    yt = np.concatenate([r["yT"] for r in res.results], axis=0)
    return np.ascontiguousarray(np.transpose(yt, (0, 2, 1)))


def kernel(**inputs):
    x = np.asarray(inputs["x"], np.float32)
    return run_layers(x, inputs, [0, 1, 2, 3], 8)
```

```python
from contextlib import ExitStack
import numpy as np
import concourse.bass as bass
import concourse.mybir as mybir
from concourse.bass_utils import run_bass_kernel_spmd

F32 = mybir.dt.float32
BF16 = mybir.dt.bfloat16
ALU = mybir.AluOpType
ACT = mybir.ActivationFunctionType
AX = mybir.AxisListType

ENGS = ("tensor", "vector", "scalar", "gpsimd", "sync")
N_DMA_SLOTS = 8

D_MODEL = 1024
NCH = 8
DEPTH = 4
N_EXPERTS = 32
ALPHA = float((2 * DEPTH) ** 0.25)
LN_EPS = 1e-5
TB = 512
NEG = -1.0e30
SKIP_MOE = False
ATT_POOL = 'gpsimd'
ATT_HP = 8


class Buf:
    __slots__ = ("name", "writer", "readers", "excl")

    def __init__(self, name="", excl=False):
        self.name = name
        self.writer = None
        self.readers = []
        self.excl = excl


class Op:
    __slots__ = ("eng", "fn", "deps", "dma", "needed", "sem", "val", "idx", "slot_prev", "epoch")

    def __init__(self, eng, fn, dma, epoch):
        self.eng = eng
        self.fn = fn
        self.dma = dma
        self.deps = []
        self.needed = False
        self.sem = None
        self.val = None
        self.idx = 0
        self.slot_prev = None
        self.epoch = epoch


class Prog:
    def __init__(self):
        self.ops = {e: [] for e in ENGS}
        self.epoch = 0
        self.dma_count = {e: 0 for e in ENGS}
        self.dma_last = {}
        self.all_bufs = []

    def buf(self, name="", excl=False):
        b = Buf(name, excl)
        self.all_bufs.append(b)
        return b

    def bufs(self, n, name=""):
        return [self.buf(f"{name}{i}") for i in range(n)]

    def add(self, eng, fn, reads=(), writes=(), dma=False, after=()):
        op = Op(eng, fn, dma, self.epoch)
        deps = {}

        def dep(o):
            if o is None or o is op:
                return
            if (not dma) and (not o.dma) and eng == "tensor" and o.eng == "tensor":
                return
            deps[id(o)] = o

        xr = [b for b in reads if b.excl and not (eng == "tensor" and not dma)]
        reads = [b for b in reads if b not in xr]
        writes = list(writes) + xr
        for b in reads:
            dep(b.writer)
        for b in writes:
            dep(b.writer)
            for r in b.readers:
                dep(r)
        for o in after:
            dep(o)
        op.deps = list(deps.values())
        for d in op.deps:
            d.needed = True
        for b in reads:
            if not dma:
                b.readers = [r for r in b.readers if r.dma or r.eng != eng]
            b.readers.append(op)
        for b in writes:
            b.writer = op
            b.readers = []
        if dma:
            n = self.dma_count[eng]
            self.dma_count[eng] = n + 1
            slot = n % N_DMA_SLOTS
            op.slot_prev = self.dma_last.get((eng, slot))
            self.dma_last[(eng, slot)] = op
            op.idx = n
        self.ops[eng].append(op)
        return op

    def barrier(self, new_epoch=False):
        lasts = []
        for e in ENGS:
            for o in reversed(self.ops[e]):
                if not o.dma:
                    lasts.append(o)
                    break
        dmas = list(self.dma_last.values())
        marks = [self.add(e, lambda eng: eng.nop(), after=lasts + dmas) for e in ENGS]
        for e in ENGS:
            self.add(e, lambda eng: eng.nop(), after=marks)
        for b in self.all_bufs:
            b.writer = None
            b.readers = []
        if new_epoch:
            self.epoch += 1

    def emit(self, block, esems, dsems):
        for e in ENGS:
            cnt = {}
            slot_uses = [0] * N_DMA_SLOTS
            for o in self.ops[e]:
                if o.dma:
                    s = o.idx % N_DMA_SLOTS
                    slot_uses[s] += 1
                    o.sem = dsems[e][s]
                    o.val = 16 * slot_uses[s]
                elif o.needed:
                    cnt[o.epoch] = cnt.get(o.epoch, 0) + 1
                    o.sem = esems[e][o.epoch]
                    o.val = cnt[o.epoch]

        def run(e):
            def body(eng):
                waited = {}
                for o in self.ops[e]:
                    need = {}
                    dl = list(o.deps)
                    if o.dma and o.slot_prev is not None:
                        dl.append(o.slot_prev)
                    for d in dl:
                        k = id(d.sem)
                        if k not in need or need[k][1] < d.val:
                            need[k] = (d.sem, d.val)
                    for k, (sem, val) in need.items():
                        if waited.get(k, 0) >= val:
                            continue
                        waited[k] = val
                        eng.wait_ge(sem, val)
                    ins = o.fn(eng)
                    if o.dma:
                        ins.then_inc(o.sem, 16)
                    elif o.needed:
                        ins.then_inc(o.sem, 1)
            return body

        block.tensor(run("tensor"))
        block.vector(run("vector"))
        block.scalar(run("scalar"))
        block.gpsimd(run("gpsimd"))
        block.sync(run("sync"))


class Carver:
    def __init__(self, pool_ap, total):
        self.ap = pool_ap
        self.total = total
        self.off = 0

    def f32(self, n):
        assert self.off + n <= self.total, ("SBUF pool overflow", self.off, n, self.total)
        v = self.ap[:, self.off:self.off + n]
        self.off += n
        return v

    def bf16(self, n):
        assert n % 2 == 0
        return self.f32(n // 2).bitcast(BF16)


def r3(ap, a):
    return ap.rearrange("p (a b) -> p a b", a=a)


def build_program(NSEQ, S, layers, dbg_after=None):
    assert S % TB == 0
    NT = S // TB
    NTL = S // 128
    nc = bass.Bass("TRN2", target_bir_lowering=False)

    def din(name, shape):
        return nc.dram_tensor(name, list(shape), F32, kind="ExternalInput").ap()

    xT = din("xT", [NSEQ, D_MODEL, S])
    yT = nc.dram_tensor("yT", [NSEQ, D_MODEL, S], F32, kind="ExternalOutput").ap()
    consts = din("consts", [128, 3 * 128 + 4 * TB])
    selc = din("selc", [32, 32 * 128])
    W = {}
    for l in layers:
        if l % 2 == 0:
            W[("win", l)] = din(f"win{l}", [D_MODEL, 2560])
            W[("wout", l)] = din(f"wout{l}", [D_MODEL, D_MODEL])
            W[("evp", l)] = din(f"evp{l}", [512, 37])
        else:
            W[("wqkv", l)] = din(f"wqkv{l}", [D_MODEL, 3072])
            W[("wo", l)] = din(f"wo{l}", [D_MODEL, D_MODEL])
        W[("lnp", l)] = din(f"lnp{l}", [D_MODEL, 4])
        W[("wr", l)] = din(f"wr{l}", [D_MODEL, 36])
        W[("br", l)] = din(f"br{l}", [1, 36])
        W[("wup", l)] = din(f"wup{l}", [N_EXPERTS, D_MODEL, 1024])
        W[("wdn", l)] = din(f"wdn{l}", [N_EXPERTS, 512, D_MODEL])

    P = Prog()
    with ExitStack() as es:
        POOLN = 53200
        pool_t = es.enter_context(nc.sbuf_tensor("pool", [128, POOLN], F32))
        psum = [es.enter_context(nc.psum_tensor(f"ps{i}", [128, TB], F32)) for i in range(8)]
        n_epochs = NSEQ + 1
        esems = {e: [es.enter_context(nc.semaphore(f"es_{e}{k}")) for k in range(n_epochs)] for e in ENGS}
        dsems = {e: [es.enter_context(nc.semaphore(f"ds_{e}{i}")) for i in range(N_DMA_SLOTS)] for e in ENGS}
        bps = [P.buf(f"ps{i}", excl=True) for i in range(8)]
        cv = Carver(pool_t, POOLN)

        xres = r3(cv.f32(NCH * S), NCH)
        xb = r3(cv.bf16(NCH * S), NCH)
        c_all = cv.f32(3 * 128)
        cU, cI, cOnes = c_all[:, 0:128], c_all[:, 128:256], c_all[:, 256:384]
        ones1024 = cv.bf16(128)
        cUb = cv.bf16(128)
        onesb = cv.bf16(128)
        ones512 = cv.bf16(128)
        b_x = P.bufs(NT, "x")
        b_xb = P.bufs(NT, "xb")
        b_const = P.buf("const")
        persist_off = cv.off

        P.add("sync", lambda e: e.dma_start(out=c_all, in_=consts[:, 0:384]), writes=[b_const], dma=True)
        P.add("gpsimd", lambda e: e.memset(ones1024, 1.0 / 1024.0), writes=[b_const])
        P.add("gpsimd", lambda e: e.tensor_copy(cUb, cU), reads=[b_const], writes=[b_const])
        P.add("gpsimd", lambda e: e.memset(onesb, 1.0), writes=[b_const])
        P.add("gpsimd", lambda e: e.memset(ones512, 1.0 / 512.0), writes=[b_const])

        def mm(out, lhsT, rhs, start, stop, reads, writes):
            return P.add("tensor", lambda e: e.matmul(out, lhsT, rhs, start=start, stop=stop),
                         reads=reads, writes=writes)

        def layer_norm(xv, bx, C, ones_bf, g_ap, b_ap, tmp, func, out_aps, out_bufs, bank_a, bank_b,
                       bf_out=None, bf_buf=None, lnb_off=0):
            lnb, b_lnb = tmp["lnb"], tmp["b_lnb"]
            m_sb, t2, rstd = tmp["m_sb"], tmp["t2"], tmp["rstd"]
            b_m, b_t2, b_rstd = tmp["b_m"], tmp["b_t2"], tmp["b_rstd"]
            lnb = lnb[:, lnb_off:lnb_off + C, :]
            lv = lnb
            pa, pb = psum[bank_a], psum[bank_b]
            P.add("scalar", lambda e: e.activation(out=lv, in_=xv, func=ACT.Copy), reads=[bx], writes=[b_lnb])
            for c in range(C):
                mm(pa[:], ones_bf, lnb[:, c, :], c == 0, c == C - 1, [b_lnb, b_const], [bps[bank_a]])
            P.add("scalar", lambda e: e.activation(out=lv, in_=xv, func=ACT.Square), reads=[bx], writes=[b_lnb])
            for c in range(C):
                mm(pb[:], ones_bf, lnb[:, c, :], c == 0, c == C - 1, [b_lnb, b_const], [bps[bank_b]])
            P.add("scalar", lambda e: e.activation(out=m_sb, in_=pa[:], func=ACT.Copy), reads=[bps[bank_a]], writes=[b_m])
            P.add("vector", lambda e: e.tensor_tensor(t2, m_sb, m_sb, op=ALU.mult), reads=[b_m], writes=[b_t2])
            P.add("vector", lambda e: e.tensor_tensor(t2, pb[:], t2, op=ALU.subtract), reads=[bps[bank_b], b_t2], writes=[b_t2])
            P.add("scalar", lambda e: e.activation(out=rstd, in_=t2, func=ACT.Ln, bias=LN_EPS), reads=[b_t2], writes=[b_rstd])
            P.add("scalar", lambda e: e.activation(out=rstd, in_=rstd, func=ACT.Exp, scale=-0.5), reads=[b_rstd], writes=[b_rstd])
            P.add("vector", lambda e: e.tensor_tensor(xv, xv, m_sb.unsqueeze(1).to_broadcast([128, C, TB]), op=ALU.subtract),
                  reads=[bx, b_m], writes=[bx])
            P.add("vector", lambda e: e.tensor_tensor(xv, xv, rstd.unsqueeze(1).to_broadcast([128, C, TB]), op=ALU.mult),
                  reads=[bx, b_rstd], writes=[bx])
            for c in range(C):
                P.add("scalar", lambda e, c=c: e.activation(out=out_aps[c], in_=xv[:, c, :], func=func,
                                                             scale=g_ap[:, c:c + 1], bias=b_ap[:, c:c + 1]),
                      reads=[bx, b_const], writes=[out_bufs[c]])
            if bf_out is not None:
                P.add("gpsimd", lambda e: e.tensor_copy(bf_out, xv), reads=[bx], writes=[bf_buf])

        def ln_tmp():
            return dict(lnb=r3(cv.bf16(NCH * TB), NCH), b_lnb=P.buf("lnb"),
                        m_sb=cv.f32(TB), t2=cv.f32(TB), rstd=cv.f32(TB),
                        b_m=P.buf("m"), b_t2=P.buf("t2"), b_rstd=P.buf("rstd"))

        def load_lnp(l):
            lnp = r3(cv.f32(NCH * 4), NCH)
            P.add("sync", lambda e: e.dma_start(out=lnp, in_=W[("lnp", l)].rearrange("(c p) k -> p c k", p=128)),
                  writes=[b_const], dma=True)
            return lnp

        def residual_ln(tb, lnp, gi, tmp, banks):
            xv = xres[:, :, tb * TB:(tb + 1) * TB]
            layer_norm(xv, b_x[tb], NCH, ones1024, lnp[:, :, gi], lnp[:, :, gi + 1], tmp, ACT.Identity,
                       [xv[:, c, :] for c in range(NCH)], [b_x[tb]] * NCH, banks[0], banks[1],
                       bf_out=xb[:, :, tb * TB:(tb + 1) * TB], bf_buf=b_xb[tb])

        def even_stage(l):
            cv.off = persist_off
            win = r3(cv.bf16(NCH * 2560), NCH)
            wout = r3(cv.bf16(NCH * 1024), NCH)
            evp = r3(cv.f32(4 * 37), 4)
            lnp = load_lnp(l)
            cvb = r3(cv.f32(4 * (TB + 2)), 4)
            gb = r3(cv.f32(4 * (TB + 30)), 4)
            gc = r3(cv.f32(4 * TB), 4)
            tmpA = [cv.f32(TB) for _ in range(2)]
            ya = [cv.f32(TB) for _ in range(2)]
            tmp = ln_tmp()
            ybuf = tmp["lnb"]
            b_y = tmp["b_lnb"]
            b_win, b_wout = P.buf("win"), P.buf("wout")
            b_cv, b_g, b_gc = P.bufs(4, "cv"), P.bufs(4, "g"), P.buf("gc")
            b_tmpA, b_ya = P.bufs(2, "tmpA"), P.bufs(2, "ya")
            winv = W[("win", l)].rearrange("(c p) n -> p c n", p=128)
            for q in range(5):
                P.add("gpsimd", lambda e, q=q: e.dma_start(out=win[:, :, q * 512:(q + 1) * 512], in_=winv[:, :, q * 512:(q + 1) * 512]),
                      writes=[b_win], dma=True)
            P.add("gpsimd", lambda e: e.dma_start(out=wout, in_=W[("wout", l)].rearrange("(c p) n -> p c n", p=128)),
                  writes=[b_wout], dma=True)
            P.add("sync", lambda e: e.dma_start(out=evp, in_=W[("evp", l)].rearrange("(c p) k -> p c k", p=128)),
                  writes=[b_const], dma=True)
            for j in range(4):
                P.add("gpsimd", lambda e, j=j: e.memset(cvb[:, j, 0:2], 0.0), writes=[b_cv[j]])
                P.add("gpsimd", lambda e, j=j: e.memset(gb[:, j, 0:30], 0.0), writes=[b_g[j]])
            cnt = [0]

            def proj(bank, col, tb):
                for c in range(NCH):
                    mm(psum[bank][:], win[:, c, col * 128:(col + 1) * 128], xb[:, c, tb * TB:(tb + 1) * TB],
                       c == 0, c == NCH - 1, [b_win, b_xb[tb]], [bps[bank]])

            for tb in range(NT):
                for j in range(4):
                    k = cnt[0] % 2
                    cnt[0] += 1
                    bA = (0, 1, 2) if k == 0 else (3, 4, 5)
                    proj(bA[0], 4 + j, tb)
                    proj(bA[1], 8 + j, tb)
                    proj(bA[2], j, tb)
                    P.add("scalar", lambda e, k=k, bA=bA: e.activation(out=tmpA[k], in_=psum[bA[0]][:], func=ACT.Copy),
                          reads=[bps[bA[0]]], writes=[b_tmpA[k]])
                    P.add("vector", lambda e, j=j, k=k, bA=bA: e.tensor_tensor(cvb[:, j, 2:2 + TB], tmpA[k], psum[bA[1]][:], op=ALU.mult),
                          reads=[b_tmpA[k], bps[bA[1]]], writes=[b_cv[j]])
                    P.add("vector", lambda e, j=j, k=k: e.tensor_scalar(ya[k], cvb[:, j, 0:TB], evp[:, j, 0:1], None, op0=ALU.mult),
                          reads=[b_cv[j], b_const], writes=[b_ya[k]])
                    for t in (1, 2):
                        P.add("vector", lambda e, j=j, k=k, t=t: e.scalar_tensor_tensor(ya[k], cvb[:, j, t:t + TB], evp[:, j, t:t + 1], ya[k],
                                                                                       op0=ALU.mult, op1=ALU.add),
                              reads=[b_cv[j], b_ya[k], b_const], writes=[b_ya[k]])
                    P.add("vector", lambda e, j=j, k=k, bA=bA: e.tensor_tensor(ybuf[:, j, :], ya[k], psum[bA[2]][:], op=ALU.mult),
                          reads=[b_ya[k], bps[bA[2]]], writes=[b_y])
                    P.add("gpsimd", lambda e, j=j: e.tensor_copy(cvb[:, j, 0:2], cvb[:, j, TB:TB + 2]), reads=[b_cv[j]], writes=[b_cv[j]])
                for j in range(4):
                    k = cnt[0] % 2
                    cnt[0] += 1
                    bB = (6, 7) if k == 0 else (0, 1)
                    proj(bB[0], 12 + j, tb)
                    proj(bB[1], 16 + j, tb)
                    P.add("scalar", lambda e, k=k, bB=bB: e.activation(out=tmpA[k], in_=psum[bB[1]][:], func=ACT.Sigmoid),
                          reads=[bps[bB[1]]], writes=[b_tmpA[k]])
                    P.add("vector", lambda e, j=j, k=k, bB=bB: e.tensor_tensor(gb[:, j, 30:30 + TB], tmpA[k], psum[bB[0]][:], op=ALU.mult),
                          reads=[b_tmpA[k], bps[bB[0]]], writes=[b_g[j]])
                    ceng = "vector"
                    P.add(ceng, lambda e, j=j: e.tensor_scalar(gc[:, j, :], gb[:, j, 0:TB], evp[:, j, 3:4], evp[:, j, 34:35],
                                                               op0=ALU.mult, op1=ALU.add),
                          reads=[b_g[j], b_const], writes=[b_gc])
                    for t in range(1, 31):
                        P.add(ceng, lambda e, j=j, t=t: e.scalar_tensor_tensor(gc[:, j, :], gb[:, j, t:t + TB], evp[:, j, 3 + t:4 + t], gc[:, j, :],
                                                                               op0=ALU.mult, op1=ALU.add),
                              reads=[b_g[j], b_gc, b_const], writes=[b_gc])
                    P.add("gpsimd", lambda e, j=j: e.tensor_copy(gb[:, j, 0:30], gb[:, j, TB:TB + 30]), reads=[b_g[j]], writes=[b_g[j]])
                layer_norm(gc, b_gc, 4, ones512, evp[:, :, 35], evp[:, :, 36], tmp, ACT.Silu,
                           [ybuf[:, 4 + c, :] for c in range(4)], [b_y] * 4, 2, 3, lnb_off=4)
                for n in range(NCH):
                    bank = 4 + (n % 2)
                    for c in range(NCH):
                        mm(psum[bank][:], wout[:, c, n * 128:(n + 1) * 128], ybuf[:, c, :], c == 0, c == NCH - 1,
                           [b_wout, b_y], [bps[bank]])
                    xs = xres[:, n, tb * TB:(tb + 1) * TB]
                    P.add("vector", lambda e, xs=xs, bank=bank: e.scalar_tensor_tensor(xs, xs, ALPHA, psum[bank][:], op0=ALU.mult, op1=ALU.add),
                          reads=[b_x[tb], bps[bank]], writes=[b_x[tb]])
                residual_ln(tb, lnp, 0, tmp, (6, 7))

        def odd_stage(l):
            cv.off = persist_off
            lnp = load_lnp(l)
            obuf = r3(cv.bf16(NCH * S), NCH)
            q_sb = cv.bf16(S)
            kz = r3(cv.bf16(2 * S), 2)
            vz = cv.bf16(NTL * 2 * 128).rearrange("p (a h n) -> p a h n", a=NTL, h=2)
            wsl = [r3(cv.bf16(NCH * 128), NCH) for _ in range(3)]
            masks = r3(cv.bf16(4 * TB), 4)
            tmp = ln_tmp()
            ssum = [cv.bf16(TB) for _ in range(2)]
            overlay_off = cv.off
            NR = 4
            e_sb = [cv.f32(TB) for _ in range(NR)]
            sp_sb = [cv.bf16(TB) for _ in range(NR)]
            z_one = cv.f32(TB)
            z_sb = [z_one for _ in range(NR)]
            ne_sb = e_sb
            a_sb = [cv.bf16(TB) for _ in range(NR)]
            b_obuf = P.bufs(NT, "obuf")
            b_q, b_k, b_v = P.buf("q"), P.buf("k"), P.buf("v")
            b_wsl = P.bufs(3, "wsl")
            b_wo, b_mask = P.buf("wo"), P.buf("mask")
            b_e, b_sp, b_a = P.bufs(NR, "e"), P.bufs(NR, "sp"), P.bufs(NR, "a")
            b_zone = P.buf("z")
            b_z = [b_zone] * NR
            b_ne = b_e
            b_ss = P.bufs(2, "ss")
            wv = W[("wqkv", l)].rearrange("(c p) n -> p c n", p=128)
            P.add("gpsimd", lambda e: e.dma_start(out=masks, in_=consts[:, 384:384 + 4 * TB].rearrange("p (a b) -> p a b", a=4)),
                  writes=[b_mask], dma=True)
            P.add("gpsimd", lambda e: e.memset(kz, 0.0), writes=[b_k])
            P.add("gpsimd", lambda e: e.memset(vz, 0.0), writes=[b_v])
            ucnt = [0]
            for hp in range(ATT_HP):
                for i3 in range(3):
                    col = i3 * 1024 + hp * 128
                    P.add("gpsimd", lambda e, i3=i3, col=col: e.dma_start(out=wsl[i3], in_=wv[:, :, col:col + 128]),
                          writes=[b_wsl[i3]], dma=True)
                for tb in range(NT):
                    ts = slice(tb * TB, (tb + 1) * TB)
                    for c in range(NCH):
                        mm(psum[0][:], wsl[0][:, c, :], xb[:, c, ts], c == 0, c == NCH - 1, [b_wsl[0], b_xb[tb]], [bps[0]])
                    P.add("scalar", lambda e, ts=ts: e.activation(out=q_sb[:, ts], in_=psum[0][:], func=ACT.Copy, scale=0.125),
                          reads=[bps[0]], writes=[b_q])
                    for c in range(NCH):
                        mm(psum[1][:], wsl[1][:, c, :], xb[:, c, ts], c == 0, c == NCH - 1, [b_wsl[1], b_xb[tb]], [bps[1]])
                    P.add("scalar", lambda e, ts=ts: e.activation(out=kz[0:64, 0, ts], in_=psum[1][0:64, :], func=ACT.Copy),
                          reads=[bps[1]], writes=[b_k])
                    P.add("scalar", lambda e, ts=ts: e.activation(out=kz[64:128, 1, ts], in_=psum[1][64:128, :], func=ACT.Copy),
                          reads=[bps[1]], writes=[b_k])
                for g4 in range(NTL // 4):
                    bank = 2
                    for k4 in range(4):
                        tl = g4 * 4 + k4
                        for c in range(NCH):
                            mm(psum[bank][:, k4 * 128:(k4 + 1) * 128], xb[:, c, tl * 128:(tl + 1) * 128], wsl[2][:, c, :],
                               c == 0, c == NCH - 1, [b_wsl[2], b_xb[tl // 4]], [bps[bank]])
                    pv = psum[bank][:].rearrange("p (a n) -> p a n", a=4)
                    P.add("vector", lambda e, g4=g4, pv=pv: e.tensor_copy(vz[:, g4 * 4:(g4 + 1) * 4, 0, 0:64], pv[:, :, 0:64]),
                          reads=[bps[bank]], writes=[b_v])
                    P.add("vector", lambda e, g4=g4, pv=pv: e.tensor_copy(vz[:, g4 * 4:(g4 + 1) * 4, 1, 64:128], pv[:, :, 64:128]),
                          reads=[bps[bank]], writes=[b_v])
                for Q in range(NT):
                    ob = 6 + (Q % 2)
                    qs = slice(Q * TB, (Q + 1) * TB)
                    nb = 4 * Q + 4
                    ulist = [(h, b) for b in range(nb - 1, -1, -1) for h in range(2)]
                    st = {}

                    def phase_a(idx):
                        h, b = ulist[idx]
                        bi = (nb - 1) - b
                        u = ucnt[0] % NR
                        zb = ucnt[0] % 3
                        cb = 3 + (ucnt[0] % 3)
                        ucnt[0] += 1
                        st[idx] = u
                        r = b - 4 * Q
                        si = h
                        mm(psum[zb][:], kz[:, h, b * 128:(b + 1) * 128], q_sb[:, qs], True, True, [b_k, b_q], [bps[zb]])
                        P.add("scalar", lambda e: e.activation(out=e_sb[u], in_=psum[zb][:], func=ACT.Exp),
                              reads=[bps[zb]], writes=[b_e[u]])
                        P.add("scalar", lambda e: e.activation(out=sp_sb[u], in_=e_sb[u], func=ACT.Ln, bias=1.0),
                              reads=[b_e[u]], writes=[b_sp[u]])
                        if r >= 0:
                            P.add(ATT_POOL, lambda e: e.tensor_tensor(sp_sb[u], sp_sb[u], masks[:, r, :], op=ALU.mult),
                                  reads=[b_sp[u], b_mask], writes=[b_sp[u]])
                        mm(psum[cb][:], cUb, sp_sb[u], True, bi == 0, [b_sp[u], b_const], [bps[cb]])
                        if bi > 0:
                            mm(psum[cb][:], onesb, ssum[si], False, True, [b_ss[si], b_const], [bps[cb]])
                        P.add("vector", lambda e: e.tensor_copy(z_sb[u], psum[zb][:]), reads=[bps[zb]], writes=[b_z[u]])
                        P.add("vector", lambda e: e.tensor_tensor(ne_sb[u], psum[cb][:], z_sb[u], op=ALU.subtract),
                              reads=[bps[cb], b_z[u]], writes=[b_ne[u]])
                        if b > 0:
                            if bi == 0:
                                P.add(ATT_POOL, lambda e: e.tensor_copy(ssum[si], sp_sb[u]), reads=[b_sp[u]], writes=[b_ss[si]])
                            else:
                                P.add(ATT_POOL, lambda e: e.tensor_tensor(ssum[si], ssum[si], sp_sb[u], op=ALU.add),
                                      reads=[b_sp[u], b_ss[si]], writes=[b_ss[si]])

                    def phase_b(idx):
                        h, b = ulist[idx]
                        u = st[idx]
                        r = b - 4 * Q
                        P.add("scalar", lambda e: e.activation(out=a_sb[u], in_=ne_sb[u], func=ACT.Exp, scale=-1.0),
                              reads=[b_ne[u]], writes=[b_a[u]])
                        if r >= 0:
                            P.add(ATT_POOL, lambda e: e.tensor_tensor(a_sb[u], a_sb[u], masks[:, r, :], op=ALU.mult),
                                  reads=[b_a[u], b_mask], writes=[b_a[u]])
                        mm(psum[ob][:], vz[:, b, h, :], a_sb[u], idx == 0, idx == len(ulist) - 1, [b_v, b_a[u]], [bps[ob]])

                    SK = 2
                    for i in range(len(ulist) + SK):
                        if i < len(ulist):
                            phase_a(i)
                        if i - SK >= 0:
                            phase_b(i - SK)
                    P.add("vector", lambda e, hp=hp, qs=qs, ob=ob: e.tensor_copy(obuf[:, hp, qs], psum[ob][:]),
                          reads=[bps[ob]], writes=[b_obuf[Q]])
            P.barrier()
            cv.off = overlay_off
            wo = r3(cv.bf16(NCH * 1024), NCH)
            P.add("gpsimd", lambda e: e.dma_start(out=wo, in_=W[("wo", l)].rearrange("(c p) n -> p c n", p=128)),
                  writes=[b_wo], dma=True)
            for tb in range(NT):
                ts = slice(tb * TB, (tb + 1) * TB)
                for n in range(NCH):
                    bank = n % 2
                    for c in range(NCH):
                        mm(psum[bank][:], wo[:, c, n * 128:(n + 1) * 128], obuf[:, c, ts], c == 0, c == NCH - 1,
                           [b_wo, b_obuf[tb]], [bps[bank]])
                    xs = xres[:, n, ts]
                    P.add("vector", lambda e, xs=xs, bank=bank: e.scalar_tensor_tensor(xs, xs, ALPHA, psum[bank][:], op0=ALU.mult, op1=ALU.add),
                          reads=[b_x[tb], bps[bank]], writes=[b_x[tb]])
                residual_ln(tb, lnp, 0, tmp, (2, 3))

        def moe_stage(l):
            cv.off = persist_off
            lnp = load_lnp(l)
            wr = r3(cv.f32(NCH * 36), NCH)
            br = cv.f32(36)
            wu = [r3(cv.bf16(NCH * 1024), NCH) for _ in range(2)]
            wd = [r3(cv.bf16(4 * 1024), 4) for _ in range(2)]
            lg = r3(cv.f32(NTL * 36), NTL)
            sel = r3(cv.f32(NTL * 32), NTL)
            oh1 = r3(cv.f32(NTL * 32), NTL)
            oh2 = r3(cv.f32(NTL * 32), NTL)
            Wt = r3(cv.f32(NTL * 32), NTL)
            ohg = r3(cv.f32(NTL * 4), NTL)
            ge = r3(cv.f32(NTL * 4), NTL)
            sm = [cv.f32(NTL) for _ in range(8)]
            selb = r3(cv.bf16(32 * 128), 32)
            WThi = cv.bf16(S)
            WTlo = cv.bf16(S)
            sg = [cv.f32(TB) for _ in range(2)]
            tt = [cv.f32(TB) for _ in range(2)]
            hb = [r3(cv.bf16(4 * TB), 4) for _ in range(2)]
            tmp = ln_tmp()
            b_wr, b_r = P.buf("wr"), P.buf("router")
            b_wu, b_wd = P.bufs(2, "wu"), P.bufs(2, "wd")
            b_sg, b_tt, b_h = P.bufs(2, "sg"), P.bufs(2, "tt"), P.bufs(2, "h")
            b_wt, b_sel = P.buf("wt"), P.buf("sel")
            P.add("sync", lambda e: e.dma_start(out=wr, in_=W[("wr", l)].rearrange("(c p) n -> p c n", p=128)), writes=[b_wr], dma=True)
            P.add("sync", lambda e: e.dma_start(out=br, in_=W[("br", l)].partition_broadcast(128)), writes=[b_wr], dma=True)
            P.add("gpsimd", lambda e: e.dma_start(out=selb[0:32, :, :], in_=selc.rearrange("k (e m) -> k e m", e=32)), writes=[b_sel], dma=True)
            TPB = 8
            for g8 in range((NTL + TPB - 1) // TPB):
                bank = g8 % 2
                nt8 = min(TPB, NTL - g8 * TPB)
                for k8 in range(nt8):
                    tl = g8 * TPB + k8
                    for c in range(NCH):
                        mm(psum[bank][:, k8 * 36:(k8 + 1) * 36], xres[:, c, tl * 128:(tl + 1) * 128], wr[:, c, :],
                           c == 0, c == NCH - 1, [b_x[tl // 4], b_wr], [bps[bank]])
                pv = psum[bank][:, 0:nt8 * 36].rearrange("p (a n) -> p a n", a=nt8)
                P.add("vector", lambda e, g8=g8, nt8=nt8, pv=pv: e.tensor_tensor(lg[:, g8 * TPB:g8 * TPB + nt8, :], pv,
                                                                               br.unsqueeze(1).to_broadcast([128, nt8, 36]), op=ALU.add),
                      reads=[bps[bank], b_wr], writes=[b_r])
            gl = lg[:, :, 0:4]
            el = lg[:, :, 4:36]
            gmax, gsum, ggate, m1, m2, dd, w1, w2 = sm

            def V(fn):
                P.add("vector", fn, reads=[b_r], writes=[b_r])

            def bc(ap, n):
                return ap.unsqueeze(2).to_broadcast([128, NTL, n])

            V(lambda e: e.reduce_max(gmax, gl, axis=AX.X))
            V(lambda e: e.tensor_tensor(ohg, gl, bc(gmax, 4), op=ALU.is_equal))
            V(lambda e: e.tensor_tensor(ge, gl, bc(gmax, 4), op=ALU.subtract))
            P.add("scalar", lambda e: e.activation(out=ge, in_=ge, func=ACT.Exp), reads=[b_r], writes=[b_r])
            V(lambda e: e.reduce_sum(gsum, ge, axis=AX.X))
            V(lambda e: e.reciprocal(ggate, gsum))
            V(lambda e: e.tensor_scalar(ohg, ohg, 1.0, -NEG, op0=ALU.subtract, op1=ALU.mult))
            V(lambda e: e.tensor_tensor(sel.rearrange("p a (g k) -> p a g k", g=4), el.rearrange("p a (g k) -> p a g k", g=4),
                                        ohg.unsqueeze(3).to_broadcast([128, NTL, 4, 8]), op=ALU.add))
            V(lambda e: e.reduce_max(m1, sel, axis=AX.X))
            V(lambda e: e.tensor_tensor(oh1, sel, bc(m1, 32), op=ALU.is_equal))
            V(lambda e: e.scalar_tensor_tensor(sel, oh1, NEG, sel, op0=ALU.mult, op1=ALU.add))
            V(lambda e: e.reduce_max(m2, sel, axis=AX.X))
            V(lambda e: e.tensor_tensor(oh2, sel, bc(m2, 32), op=ALU.is_equal))
            V(lambda e: e.tensor_tensor(dd, m2, m1, op=ALU.subtract))
            P.add("scalar", lambda e: e.activation(out=dd, in_=dd, func=ACT.Exp), reads=[b_r], writes=[b_r])
            V(lambda e: e.tensor_scalar(w1, dd, 1.0, None, op0=ALU.add))
            V(lambda e: e.reciprocal(w1, w1))
            V(lambda e: e.tensor_tensor(w2, dd, w1, op=ALU.mult))
            V(lambda e: e.tensor_tensor(w1, w1, ggate, op=ALU.mult))
            V(lambda e: e.tensor_tensor(w2, w2, ggate, op=ALU.mult))
            V(lambda e: e.tensor_tensor(oh1, oh1, bc(w1, 32), op=ALU.mult))
            V(lambda e: e.tensor_tensor(oh2, oh2, bc(w2, 32), op=ALU.mult))
            V(lambda e: e.tensor_tensor(Wt, oh1, oh2, op=ALU.add))
            for tb in range(NT):
                xv = xres[:, :, tb * TB:(tb + 1) * TB]
                P.add("scalar", lambda e, xv=xv: e.mul(xv, xv, ALPHA), reads=[b_x[tb]], writes=[b_x[tb]])
            for tb in range(NT):
                ts = slice(tb * TB, (tb + 1) * TB)
                bank = 2 + (tb % 2)
                for k4 in range(4):
                    tl = tb * 4 + k4
                    mm(psum[bank][0:32, k4 * 128:(k4 + 1) * 128], Wt[:, tl, :], cI, True, True, [b_r, b_const], [bps[bank]])
                P.add("scalar", lambda e, ts=ts, bank=bank: e.activation(out=WThi[0:32, ts], in_=psum[bank][0:32, :], func=ACT.Copy),
                      reads=[bps[bank]], writes=[b_wt])
                P.add("vector", lambda e, ts=ts, bank=bank: e.tensor_tensor(WTlo[0:32, ts], psum[bank][0:32, :], WThi[0:32, ts], op=ALU.subtract),
                      reads=[bps[bank], b_wt], writes=[b_wt])
            wupv = W[("wup", l)]
            wdnv = W[("wdn", l)]
            items = [(ex, tb) for ex in range(N_EXPERTS) for tb in range(NT)]

            def stage_up(i):
                ex, tb = items[i]
                ws = ex % 2
                u = i % 2
                ts = slice(tb * TB, (tb + 1) * TB)
                if tb == 0:
                    P.add("gpsimd", lambda e: e.dma_start(out=wu[ws], in_=wupv[ex].rearrange("(c p) n -> p c n", p=128)),
                          writes=[b_wu[ws]], dma=True)
                    P.add("gpsimd", lambda e: e.dma_start(out=wd[ws], in_=wdnv[ex].rearrange("(c p) n -> p c n", p=128)),
                          writes=[b_wd[ws]], dma=True)
                wbk = u
                mm(psum[wbk][:], selb[0:32, ex, :], WThi[0:32, ts], True, False, [b_wt, b_sel], [bps[wbk]])
                mm(psum[wbk][:], selb[0:32, ex, :], WTlo[0:32, ts], False, True, [b_wt, b_sel], [bps[wbk]])
                for j in range(4):
                    gbk = 2 + 2 * (j % 2)
                    ubk = gbk + 1
                    for c in range(NCH):
                        mm(psum[gbk][:], wu[ws][:, c, j * 128:(j + 1) * 128], xb[:, c, ts], c == 0, c == NCH - 1,
                           [b_wu[ws], b_xb[tb]], [bps[gbk]])
                    for c in range(NCH):
                        mm(psum[ubk][:], wu[ws][:, c, 512 + j * 128:512 + (j + 1) * 128], xb[:, c, ts], c == 0, c == NCH - 1,
                           [b_wu[ws], b_xb[tb]], [bps[ubk]])
                    k = j % 2
                    P.add("scalar", lambda e, k=k, gbk=gbk: e.activation(out=sg[k], in_=psum[gbk][:], func=ACT.Silu),
                          reads=[bps[gbk]], writes=[b_sg[k]])
                    P.add("vector", lambda e, k=k, ubk=ubk: e.tensor_tensor(tt[k], sg[k], psum[ubk][:], op=ALU.mult),
                          reads=[b_sg[k], bps[ubk]], writes=[b_tt[k]])
                    P.add("vector", lambda e, k=k, j=j: e.tensor_tensor(hb[u][:, j, :], tt[k], psum[wbk][:], op=ALU.mult),
                          reads=[b_tt[k], bps[wbk]], writes=[b_h[u]])

            def stage_down(i):
                ex, tb = items[i]
                ws = ex % 2
                u = i % 2
                ts = slice(tb * TB, (tb + 1) * TB)
                for n in range(NCH):
                    ybk = 6 + (n % 2)
                    for j in range(4):
                        mm(psum[ybk][:], wd[ws][:, j, n * 128:(n + 1) * 128], hb[u][:, j, :], j == 0, j == 3,
                           [b_wd[ws], b_h[u]], [bps[ybk]])
                    xs = xres[:, n, ts]
                    P.add("vector", lambda e, xs=xs, ybk=ybk: e.tensor_tensor(xs, xs, psum[ybk][:], op=ALU.add),
                          reads=[b_x[tb], bps[ybk]], writes=[b_x[tb]])

            stage_up(0)
            for i in range(len(items)):
                if i + 1 < len(items):
                    stage_up(i + 1)
                stage_down(i)
            for tb in range(NT):
                residual_ln(tb, lnp, 2, tmp, (2, 3))

        for s in range(NSEQ):
            for c in range(NCH):
                P.add("sync", lambda e, s=s, c=c: e.dma_start(out=xres[:, c, :], in_=xT[s, c * 128:(c + 1) * 128, :]),
                      writes=b_x, dma=True)
            for tb in range(NT):
                ts = slice(tb * TB, (tb + 1) * TB)
                P.add("gpsimd", lambda e, ts=ts: e.tensor_copy(xb[:, :, ts], xres[:, :, ts]), reads=[b_x[tb]], writes=[b_xb[tb]])
            for l in layers:
                if l % 2 == 0:
                    even_stage(l)
                else:
                    odd_stage(l)
                P.barrier()
                if not SKIP_MOE:
                    moe_stage(l)
                    P.barrier()
            for c in range(NCH):
                P.add("sync", lambda e, s=s, c=c: e.dma_start(out=yT[s, c * 128:(c + 1) * 128, :], in_=xres[:, c, :]),
                      reads=b_x, writes=[], dma=True)
            P.barrier(new_epoch=True)
        with nc.Block() as block:
            P.emit(block, esems, dsems)
    return nc


def make_consts():
    j = np.arange(128)[:, None]
    s = np.arange(128)[None, :]
    U = (j >= s).astype(np.float32)
    I = np.eye(128, dtype=np.float32)
    ones = np.ones((128, 128), np.float32)
    t = np.arange(TB)[None, :]
    masks = [(t > (r * 128 + j)).astype(np.float32) for r in range(4)]
    return np.ascontiguousarray(np.concatenate([U, I, ones] + masks, axis=1))


def layer_inputs(inp, layers):
    f = np.float32
    d = {"consts": make_consts(),
         "selc": np.ascontiguousarray(np.repeat(np.eye(32, dtype=np.float32), 128, axis=1))}
    for l in layers:
        i = l // 2
        if l % 2 == 0:
            d[f"win{l}"] = np.ascontiguousarray(inp["even_w_in"][i], f)
            d[f"wout{l}"] = np.ascontiguousarray(inp["even_w_out"][i], f)
            d[f"evp{l}"] = np.ascontiguousarray(np.concatenate(
                [inp["even_conv_a"][i].T, inp["even_conv_b_w"][i].T, inp["even_conv_b_bias"][i][:, None],
                 inp["even_norm_b_g"][i][:, None], inp["even_norm_b_b"][i][:, None]], axis=1), f)
        else:
            d[f"wqkv{l}"] = np.ascontiguousarray(inp["odd_w_qkv"][i], f)
            d[f"wo{l}"] = np.ascontiguousarray(inp["odd_w_o"][i], f)
        d[f"lnp{l}"] = np.ascontiguousarray(np.stack(
            [inp["ln_mix_g"][l], inp["ln_mix_b"][l], inp["ln_ffn_g"][l], inp["ln_ffn_b"][l]], axis=1), f)
        d[f"wr{l}"] = np.ascontiguousarray(np.concatenate([inp["router_group_w"][l], inp["router_expert_w"][l]], axis=1), f)
        d[f"br{l}"] = np.ascontiguousarray(np.concatenate([inp["router_group_b"][l], inp["router_expert_b"][l]])[None, :], f)
        d[f"wup{l}"] = np.ascontiguousarray(inp["expert_w_up"][l], f)
        d[f"wdn{l}"] = np.ascontiguousarray(inp["expert_w_down"][l], f)
    return d


def run_layers(x, inp, layers, n_cores):
    B, S, _ = x.shape
    assert B % n_cores == 0
    nseq = B // n_cores
    nc = build_program(nseq, S, layers)
    shared = layer_inputs(inp, layers)
    xt = np.ascontiguousarray(np.transpose(np.asarray(x, np.float32), (0, 2, 1)))
    in_maps = []
    for c in range(n_cores):
        m = dict(shared)
        m["xT"] = xt[c * nseq:(c + 1) * nseq]
        in_maps.append(m)
    res = run_bass_kernel_spmd(nc, in_maps, core_ids=list(range(n_cores)))
    yt = np.concatenate([r["yT"] for r in res.results], axis=0)
    return np.ascontiguousarray(np.transpose(yt, (0, 2, 1)))


def kernel(**inputs):
    x = np.asarray(inputs["x"], np.float32)
    return run_layers(x, inputs, [0, 1, 2, 3], 8)
```
